# Optimizing a Trainium2 kernel written in Bass

```python
import math
import jax, jax.numpy as jnp
from jax import lax
import numpy as np

D_MODEL = 1024
BATCH = 8
SEQ = 2048
DEPTH = 1

CHUNK = 64
Q_BLOCK = 128
NORM_EPS = 1e-5
POOL_WIDTH = D_MODEL // 2
POOL_WINDOWS = (2, 4, 8, 16)
POOL_GROUPS = len(POOL_WINDOWS)
POOL_GROUP_DIM = POOL_WIDTH // POOL_GROUPS
DIFF_HEADS = 4
DIFF_HEAD_DIM = 64
DIFF_V_DIM = 2 * DIFF_HEAD_DIM
DIFF_WIDTH = DIFF_HEADS * DIFF_V_DIM
QK_WIDTH = DIFF_HEADS * 2 * DIFF_HEAD_DIM
MIX_WIDTH = POOL_WIDTH + DIFF_WIDTH
IN_WIDTH = POOL_WIDTH + 2 * QK_WIDTH + DIFF_WIDTH
ROT_DIM = DIFF_HEAD_DIM // 4
ROPE_THETA = 500000.0
MEM_LEN = 256
X_HEADS = 4
X_HEAD_DIM = D_MODEL // X_HEADS
N_EXPERTS = 32
TOP_K = 4
D_EXPERT = D_MODEL
SWIGLU_ALPHA = 1.702
SWIGLU_LIMIT = 7.0
EXPERT_BLOCK = 128

kernel_name = 'hybrid_pool_diffattn_moe_block'


def rms_norm(x, g):
    xf = x.astype(jnp.float32)
    y = xf * lax.rsqrt(jnp.mean(xf * xf, axis=-1, keepdims=True) + NORM_EPS)
    return (y * g.astype(jnp.float32)).astype(x.dtype)


def multiscale_pool(u, w_pool, pool_scale):
    B, S, _ = u.shape
    uf = u.astype(jnp.float32).reshape(B, S, POOL_GROUPS, POOL_GROUP_DIM)
    csum = jnp.pad(jnp.cumsum(uf, axis=1), ((0, 0), (1, 0), (0, 0), (0, 0)))
    t = jnp.arange(S)[:, None]
    win = jnp.array(POOL_WINDOWS, dtype=jnp.int32)[None, :]
    lo = jnp.maximum(t + 1 - win, 0)
    gidx = jnp.arange(POOL_GROUPS)[None, :]
    window_sum = csum[:, 1:] - csum[:, lo, gidx]
    count = (t + 1 - lo).astype(jnp.float32)
    mixed = (window_sum / count[None, :, :, None] - uf).astype(u.dtype)
    y = jnp.einsum('bsgc,gcd->bsgd', mixed, w_pool)
    y = y * pool_scale.reshape(POOL_GROUPS, POOL_GROUP_DIM)
    return y.reshape(B, S, POOL_WIDTH)


def rotary_tables(positions):
    inv_freq = ROPE_THETA ** (-jnp.arange(0, ROT_DIM, 2, dtype=jnp.float32) / ROT_DIM)
    ang = positions.astype(jnp.float32)[..., None] * inv_freq
    return jnp.cos(ang)[:, :, None, None, :], jnp.sin(ang)[:, :, None, None, :]


def partial_rotary(x, cos, sin):
    half = ROT_DIM // 2
    xr = x[..., :ROT_DIM].astype(jnp.float32)
    x1, x2 = xr[..., :half], xr[..., half:]
    rot = jnp.concatenate([x1 * cos - x2 * sin, x2 * cos + x1 * sin], axis=-1)
    return jnp.concatenate([rot.astype(x.dtype), x[..., ROT_DIM:]], axis=-1)


def diff_attention(q, k, v, lam):
    S = q.shape[3]
    scale = DIFF_HEAD_DIM ** -0.5
    outs = []
    for qb in range(S // Q_BLOCK):
        q0 = qb * Q_BLOCK
        kend = q0 + Q_BLOCK
        s = jnp.einsum('bhmqd,bhmkd->bhmqk', q[:, :, :, q0:kend], k[:, :, :, :kend],
                       preferred_element_type=jnp.float32) * scale
        q_chunk = (q0 + jnp.arange(Q_BLOCK)) // CHUNK
        k_chunk = jnp.arange(kend) // CHUNK
        mask = k_chunk[None, :] <= q_chunk[:, None]
        p = jax.nn.softmax(jnp.where(mask, s, -jnp.inf), axis=-1)
        a = p[:, :, 0] - lam * p[:, :, 1]
        outs.append(jnp.einsum('bhqk,bhkv->bhqv', a.astype(v.dtype), v[:, :, :kend]))
    return jnp.concatenate(outs, axis=2)


def cross_attention(h, mem_n, w_cq, w_ckv, w_co):
    B, S, _ = h.shape
    M = mem_n.shape[1]
    q = (h @ w_cq).reshape(B, S, X_HEADS, X_HEAD_DIM)
    kv = (mem_n @ w_ckv).reshape(B, M, 2, X_HEADS, X_HEAD_DIM)
    k, v = kv[:, :, 0], kv[:, :, 1]
    s = jnp.einsum('bshd,bmhd->bhsm', q, k, preferred_element_type=jnp.float32) * (X_HEAD_DIM ** -0.5)
    p = jax.nn.softmax(s, axis=-1)
    o = jnp.einsum('bhsm,bmhd->bshd', p.astype(v.dtype), v).reshape(B, S, D_MODEL)
    return o @ w_co


def clamped_swiglu(gu):
    gate, up = gu[..., :D_EXPERT], gu[..., D_EXPERT:]
    gate = jnp.minimum(gate, SWIGLU_LIMIT)
    up = jnp.clip(up, -SWIGLU_LIMIT, SWIGLU_LIMIT)
    return (up + 1.0) * gate * jax.nn.sigmoid(SWIGLU_ALPHA * gate)


def moe_ffn(h, w_router, b_router, w_gu, b_gu, w_down, b_down):
    B, S, D = h.shape
    T = B * S
    xt = h.reshape(T, D)
    logits = (xt @ w_router + b_router).astype(jnp.float32)
    top_val, top_idx = lax.top_k(logits, TOP_K)
    gates = jax.nn.softmax(top_val, axis=-1)
    flat_e = top_idx.reshape(-1)
    flat_tok = jnp.repeat(jnp.arange(T, dtype=jnp.int32), TOP_K)
    flat_g = gates.reshape(-1)
    order = jnp.argsort(flat_e)
    se = flat_e[order]
    counts = jnp.bincount(flat_e, length=N_EXPERTS)
    start = jnp.cumsum(counts) - counts
    pcounts = (counts + EXPERT_BLOCK - 1) // EXPERT_BLOCK * EXPERT_BLOCK
    pend = jnp.cumsum(pcounts)
    pstart = pend - pcounts
    dest = pstart[se] + jnp.arange(T * TOP_K) - start[se]
    n_rows = T * TOP_K + N_EXPERTS * EXPERT_BLOCK
    n_blocks = n_rows // EXPERT_BLOCK
    row_tok = jnp.zeros((n_rows,), jnp.int32).at[dest].set(flat_tok[order])
    row_gate = jnp.zeros((n_rows,), jnp.float32).at[dest].set(flat_g[order])
    block_e = jnp.minimum(jnp.searchsorted(pend, jnp.arange(n_blocks) * EXPERT_BLOCK, side='right'),
                          N_EXPERTS - 1)

    def expert_block(args):
        tok, g, e = args
        xb = xt[tok]
        hb = clamped_swiglu(xb @ w_gu[e] + b_gu[e])
        yb = hb @ w_down[e] + b_down[e]
        return yb * g[:, None].astype(yb.dtype)

    y_rows = lax.map(expert_block, (row_tok.reshape(n_blocks, EXPERT_BLOCK),
                                    row_gate.reshape(n_blocks, EXPERT_BLOCK), block_e))
    y = jnp.zeros((T, D), h.dtype).at[row_tok].add(y_rows.reshape(n_rows, D).astype(h.dtype))
    return y.reshape(B, S, D)


def setup_inputs(seed: int = 0) -> dict:
    key = jax.random.key(seed)
    ks = jax.random.split(key, 32)
    f32 = jnp.float32
    L = DEPTH

    def nrm(k, shape, scale):
        return jax.random.normal(k, shape, f32) * scale

    def gain(k, shape):
        return 1.0 + 0.02 * jax.random.normal(k, shape, f32)

    offsets = jax.random.randint(ks[1], (BATCH, 1), 0, 4096, dtype=jnp.int32)
    return {
        'x': nrm(ks[0], (BATCH, SEQ, D_MODEL), 1.0),
        'positions': offsets + jnp.arange(SEQ, dtype=jnp.int32)[None, :],
        'mem': nrm(ks[2], (BATCH, MEM_LEN, D_MODEL), 1.0),
        'attn_norm_g': gain(ks[3], (L, D_MODEL)),
        'w_in': nrm(ks[4], (L, D_MODEL, IN_WIDTH), D_MODEL ** -0.5),
        'w_pool': nrm(ks[5], (L, POOL_GROUPS, POOL_GROUP_DIM, POOL_GROUP_DIM), POOL_GROUP_DIM ** -0.5),
        'pool_scale': gain(ks[6], (L, POOL_WIDTH)),
        'lambda_q1': nrm(ks[7], (L, DIFF_HEAD_DIM), 0.1),
        'lambda_k1': nrm(ks[8], (L, DIFF_HEAD_DIM), 0.1),
        'lambda_q2': nrm(ks[9], (L, DIFF_HEAD_DIM), 0.1),
        'lambda_k2': nrm(ks[10], (L, DIFF_HEAD_DIM), 0.1),
        'subln_g': gain(ks[11], (L, DIFF_V_DIM)),
        'w_out': nrm(ks[12], (L, MIX_WIDTH, D_MODEL), MIX_WIDTH ** -0.5),
        'xattn_norm_g': gain(ks[13], (L, D_MODEL)),
        'mem_norm_g': gain(ks[14], (L, D_MODEL)),
        'w_cq': nrm(ks[15], (L, D_MODEL, D_MODEL), D_MODEL ** -0.5),
        'w_ckv': nrm(ks[16], (L, D_MODEL, 2 * D_MODEL), D_MODEL ** -0.5),
        'w_co': nrm(ks[17], (L, D_MODEL, D_MODEL), D_MODEL ** -0.5),
        'ffn_norm_g': gain(ks[18], (L, D_MODEL)),
        'w_router': nrm(ks[19], (L, D_MODEL, N_EXPERTS), D_MODEL ** -0.5),
        'b_router': nrm(ks[20], (L, N_EXPERTS), 0.01),
        'w_gu': nrm(ks[21], (L, N_EXPERTS, D_MODEL, 2 * D_EXPERT), D_MODEL ** -0.5),
        'b_gu': nrm(ks[22], (L, N_EXPERTS, 2 * D_EXPERT), 0.01),
        'w_down': nrm(ks[23], (L, N_EXPERTS, D_EXPERT, D_MODEL), D_EXPERT ** -0.5),
        'b_down': nrm(ks[24], (L, N_EXPERTS, D_MODEL), 0.01),
        'final_norm_g': gain(ks[25], (D_MODEL,)),
    }


def reference(x, positions, mem, attn_norm_g, w_in, w_pool, pool_scale, lambda_q1, lambda_k1,
              lambda_q2, lambda_k2, subln_g, w_out, xattn_norm_g, mem_norm_g, w_cq, w_ckv, w_co,
              ffn_norm_g, w_router, b_router, w_gu, b_gu, w_down, b_down, final_norm_g):
    B, S, _ = x.shape
    cos, sin = rotary_tables(positions)
    for l in range(DEPTH):
        h = rms_norm(x, attn_norm_g[l])
        u = h @ w_in[l]
        u_pool, u_q, u_k, u_v = jnp.split(
            u, [POOL_WIDTH, POOL_WIDTH + QK_WIDTH, POOL_WIDTH + 2 * QK_WIDTH], axis=-1)
        y_pool = multiscale_pool(u_pool, w_pool[l], pool_scale[l])

        q = partial_rotary(u_q.reshape(B, S, DIFF_HEADS, 2, DIFF_HEAD_DIM), cos, sin)
        k = partial_rotary(u_k.reshape(B, S, DIFF_HEADS, 2, DIFF_HEAD_DIM), cos, sin)
        q = q.transpose(0, 2, 3, 1, 4)
        k = k.transpose(0, 2, 3, 1, 4)
        v = u_v.reshape(B, S, DIFF_HEADS, DIFF_V_DIM).transpose(0, 2, 1, 3)
        lam_init = 0.8 - 0.6 * math.exp(-0.3 * l)
        lam = (jnp.exp(jnp.sum(lambda_q1[l].astype(jnp.float32) * lambda_k1[l].astype(jnp.float32)))
               - jnp.exp(jnp.sum(lambda_q2[l].astype(jnp.float32) * lambda_k2[l].astype(jnp.float32)))
               + lam_init)
        o = diff_attention(q, k, v, lam)
        o = rms_norm(o, subln_g[l]) * (1.0 - lam_init)
        y_diff = o.transpose(0, 2, 1, 3).reshape(B, S, DIFF_WIDTH)
        x = x + jnp.concatenate([y_pool, y_diff], axis=-1) @ w_out[l]

        x = x + cross_attention(rms_norm(x, xattn_norm_g[l]), rms_norm(mem, mem_norm_g[l]),
                                w_cq[l], w_ckv[l], w_co[l])

        x = x + moe_ffn(rms_norm(x, ffn_norm_g[l]), w_router[l], b_router[l], w_gu[l], b_gu[l],
                        w_down[l], b_down[l])
    return rms_norm(x, final_norm_g)
```

```python
from contextlib import ExitStack
import math
import numpy as np
import concourse.bass as bass
import concourse.mybir as mybir
from concourse.bass_utils import run_bass_kernel_spmd

F32 = mybir.dt.float32
BF16 = mybir.dt.bfloat16
I32 = mybir.dt.int32
AF = mybir.ActivationFunctionType
ALU = mybir.AluOpType
AX = mybir.AxisListType

NCORES = 8
S = 2048
D = 1024
NT = S // 128
MEM = 256
NE = 32
CAP = 512
NST = CAP // 128
PROFILE = [512] * 5 + [448] * 7 + [384] * 6 + [320] * 9 + [256] * 5
ITEM_TILES = [(c + 127) // 128 for c in PROFILE]
ITEM_BASE = [sum(ITEM_TILES[:j]) * 128 for j in range(NE)]
NROWS = sum(ITEM_TILES) * 128
EPS = 1e-5
LAM_INIT = 0.8 - 0.6 * math.exp(0.0)
TWO_PI = 2.0 * math.pi
SWIGLU_ALPHA = 1.702
SWIGLU_LIMIT = 7.0


class Sem:
    def __init__(self, h):
        self.h = h
        self.n = 0


class Prog:
    ENGS = ("sync", "scalar", "vector", "gpsimd", "tensor")
    _uid = 0

    def __init__(self, nc, es):
        self.nc = nc
        self.es = es
        Prog._uid += 1
        self.tag = f"p{Prog._uid}"
        self.q = {e: [] for e in self.ENGS}
        self.esem = {}
        for e in ("scalar", "vector", "gpsimd", "tensor"):
            self.esem[e] = Sem(es.enter_context(nc.semaphore(f"{self.tag}_{e}")))
        self.waited = {e: {} for e in self.ENGS}
        self.nds = 0

    def dsem(self):
        self.nds += 1
        return Sem(self.es.enter_context(self.nc.semaphore(f"{self.tag}_d{self.nds}")))

    def _filter(self, eng, waits):
        out = []
        w = self.waited[eng]
        for tok in waits:
            if tok is None:
                continue
            s, v = tok
            if w.get(id(s), 0) >= v:
                continue
            w[id(s)] = v
            out.append((s, v))
        return out

    def op(self, eng, meth, *args, waits=(), sig=True, **kw):
        ws = self._filter(eng, waits)
        sem = self.esem[eng]
        tok = None
        if sig:
            sem.n += 1
            tok = (sem, sem.n)

        def run(e):
            for (s, v) in ws:
                e.wait_ge(s.h, v)
            inst = getattr(e, meth)(*args, **kw)
            if sig:
                inst.then_inc(sem.h, 1)
        self.q[eng].append(run)
        return tok

    def dma(self, eng, ds, out, in_, waits=(), meth="dma_start", **kw):
        ws = self._filter(eng, waits)
        ds.n += 16
        tok = (ds, ds.n)

        def run(e):
            for (s, v) in ws:
                e.wait_ge(s.h, v)
            getattr(e, meth)(out=out, in_=in_, **kw).then_inc(ds.h, 16)
        self.q[eng].append(run)
        return tok

    def wait(self, eng, waits):
        ws = self._filter(eng, waits)

        def run(e):
            for (s, v) in ws:
                e.wait_ge(s.h, v)
        self.q[eng].append(run)

    def run(self):
        q = self.q
        with self.nc.Block() as blk:
            @blk.sync
            def _(e):
                for f in q["sync"]:
                    f(e)

            @blk.scalar
            def _(e):
                for f in q["scalar"]:
                    f(e)

            @blk.vector
            def _(e):
                for f in q["vector"]:
                    f(e)

            @blk.gpsimd
            def _(e):
                for f in q["gpsimd"]:
                    f(e)

            @blk.tensor
            def _(e):
                for f in q["tensor"]:
                    f(e)


class Ring:
    def __init__(self, bufs):
        self.bufs = bufs
        self.free = [None] * len(bufs)
        self.n = 0

    def get(self):
        i = self.n % len(self.bufs)
        self.n += 1
        return i, self.bufs[i], self.free[i]

    def release(self, i, tok):
        self.free[i] = tok


def _rstd(P, ss_ap, out_ap, n, waits, eps_t):
    t = P.op("scalar", "activation", out=out_ap, in_=ss_ap, func=AF.Ln, bias=eps_t, scale=1.0 / n, waits=waits)
    return P.op("scalar", "activation", out=out_ap, in_=out_ap, func=AF.Exp, scale=-0.5, waits=[t])


def _sincos(P, ang, out_sin, out_cos, tmpf, tmpi, waits):
    prev = []
    for (shift, dst) in ((0.0, out_sin), (0.5 * math.pi, out_cos)):
        t = P.op("vector", "tensor_scalar", tmpf[:], ang[:], shift, 1.0 / TWO_PI, ALU.add, ALU.mult, waits=list(waits) + prev)
        t = P.op("vector", "tensor_copy", tmpi[:], tmpf[:], waits=[t])
        t = P.op("vector", "tensor_copy", tmpf[:], tmpi[:], waits=[t])
        t = P.op("vector", "scalar_tensor_tensor", out=tmpf[:], in0=tmpf[:], scalar=-TWO_PI, in1=ang[:],
                 op0=ALU.mult, op1=ALU.add, waits=[t])
        t = P.op("vector", "tensor_scalar", tmpf[:], tmpf[:], shift, -math.pi, ALU.add, ALU.max, waits=[t])
        t = P.op("vector", "tensor_scalar_min", tmpf[:], tmpf[:], math.pi, waits=[t])
        t = P.op("scalar", "activation", out=dst[:], in_=tmpf[:], func=AF.Sin, waits=[t])
        prev = [t]
    return prev[0]


def build(stage=99):
    nc = bass.Bass("TRN2", target_bir_lowering=False)

    def din(name, shape, dt=F32):
        return nc.dram_tensor(name, list(shape), dt, kind="ExternalInput").ap()

    x_d = din("x", [S, D])
    pos_d = din("pos", [128, NT], I32)
    mem_d = din("mem", [MEM, D])
    attn_g_d = din("attn_norm_g", [1, D])
    w_in_d = din("w_in", [D, 2048])
    w_pool_d = din("w_pool", [4, 128, 128])
    pool_scale_d = din("pool_scale", [128, 4])
    lam_d = din("lam4", [1, 256])
    subln_g_d = din("subln_g", [1, 128])
    w_out_d = din("w_out", [D, D])
    xattn_g_d = din("xattn_norm_g", [1, D])
    mem_g_d = din("mem_norm_g", [1, D])
    w_cq_d = din("w_cq", [D, D])
    w_ckv_d = din("w_ckv", [D, 2 * D])
    w_co_d = din("w_co", [D, D])
    ffn_g_d = din("ffn_norm_g", [1, D])
    w_router_d = din("w_router", [D, NE])
    b_router_d = din("b_router", [1, NE])
    w_gu_d = din("w_gu", [NE, D, 2 * D])
    b_gu_d = din("b_gu", [NE * 16, 128])
    w_down_d = din("w_down", [NE, D, D])
    b_down_d = din("b_down", [NE, D])
    final_g_d = din("final_norm_g", [1, D])
    ident_d = din("c_ident", [128, 128])
    invf_d = din("c_invf", [1, 8])
    poolm_d = din("c_poolm", [3, 128, 4, 128])
    tri_d = din("c_tri", [128, 128])
    eoff_d = din("c_eoff", [1, NE])
    trash_d = din("c_trash", [128, 1])
    lt_d = din("c_lt", [1, NE * NE])
    tab_d = din("c_tab", [1, 3 * NE])
    iotacp_d = din("c_iotacp", [128, 8])

    out_d = nc.dram_tensor("out", [S, D], F32, kind="ExternalOutput").ap()
    dbg = {}
    if stage < 99:
        dbg["u"] = nc.dram_tensor("dbg_u", [S, 2048], F32, kind="ExternalOutput").ap()
        dbg["qk"] = nc.dram_tensor("dbg_qk", [128, 8 * S], F32, kind="ExternalOutput").ap()
        dbg["x"] = nc.dram_tensor("dbg_x", [S, D], F32, kind="ExternalOutput").ap()

    xd_d = nc.dram_tensor("xd_scr", [NROWS + 128, D], BF16, kind="Internal").ap()
    top = ExitStack()
    with top:
        def sb(name, shape, dt, es=top):
            return es.enter_context(nc.sbuf_tensor(name, list(shape), dt))

        def ps(name, shape, dt, es=top):
            return es.enter_context(nc.psum_tensor(name, list(shape), dt))

        x_res = sb("x_res", [128, NT, D], F32)
        ident_f = sb("ident_f", [128, 128], F32)
        ident_b = sb("ident_b", [128, 128], BF16)
        ones_b = sb("ones_b", [128, 128], BF16)
        eps_t = sb("eps_t", [128, 1], F32)

        p1 = ExitStack()
        with p1:
            upool = sb("upool", [128, NT, 512], BF16, p1)
            qkT = sb("qkT", [128, 8, S], BF16, p1)
            v_all = sb("v_all", [128, NT, 4, 132], BF16, p1)
            mixT_w = sb("mixT_w", [128, 8, 2048], BF16, p1)
            w_in_sb = mixT_w

            pa = ExitStack()
            with pa:
                P = Prog(nc, pa)
                g_bc = sb("g_bc", [128, D], F32, pa)
                pos_i = sb("pos_i", [128, NT], I32, pa)
                pos_f = sb("pos_f", [128, NT], F32, pa)
                invf = sb("invf", [128, 8], F32, pa)
                ang = sb("ang", [128, NT, 8], F32, pa)
                argt = sb("argt", [128, NT, 8], F32, pa)
                argi = sb("argi", [128, NT, 8], I32, pa)
                cos_t = sb("cos_t", [128, NT, 8], F32, pa)
                sin_t = sb("sin_t", [128, NT, 8], F32, pa)
                ss = sb("ss", [128, NT], F32, pa)
                rstd = sb("rstd", [128, NT], F32, pa)
                junk = sb("junk", [128, D], BF16, pa)
                h_tok = [sb(f"h_tok{i}", [128, D], BF16, pa) for i in range(2)]
                hT = [sb(f"hT{i}", [128, 8, 128], BF16, pa) for i in range(2)]
                qk_rot = [sb(f"qk_rot{i}", [128, 1024], BF16, pa) for i in range(2)]
                rt = [sb(f"rt{i}", [128, 16, 8], F32, pa) for i in range(4)]
                udbg = sb("udbg", [128, 2048], F32, pa) if stage == 1 else None
                tp_ps = [ps(f"tp_ps{i}", [128, 8, 128], BF16, pa) for i in range(2)]
                u_ps = ps("u_ps", [128, 2048], F32, pa)
                qt_ps = ps("qt_ps", [128, 8, 128], BF16, pa)

                d_c = P.dsem()
                d_w = P.dsem()
                d_x = [P.dsem() for _ in range(4)]
                d_o = P.dsem()

                P.dma("sync", d_c, ident_f[:], ident_d)
                P.dma("sync", d_c, g_bc[:], attn_g_d.partition_broadcast(128))
                P.dma("sync", d_c, pos_i[:], pos_d)
                t_c = P.dma("sync", d_c, invf[:], invf_d.partition_broadcast(128))
                w_in_v = w_in_d.rearrange("(c p) f -> p c f", p=128)
                d_wn = [P.dsem() for _ in range(4)]
                t_wn = [P.dma("gpsimd", d_wn[nb], w_in_sb[:, :, nb * 512:(nb + 1) * 512], w_in_v[:, :, nb * 512:(nb + 1) * 512])
                        for nb in range(4)]
                t_idb = P.op("vector", "tensor_copy", ident_b[:], ident_f[:], waits=[t_c])
                P.op("vector", "memset", ones_b[:], 1.0)
                t_v1 = P.op("vector", "memset", v_all[:, :, :, 128:132], 1.0)
                t_eps = P.op("vector", "memset", eps_t[:], EPS)
                t = P.op("vector", "tensor_copy", pos_f[:], pos_i[:], waits=[t_c])
                t = P.op("vector", "tensor_tensor", ang[:], pos_f[:].unsqueeze(2).to_broadcast([128, NT, 8]),
                         invf[:].unsqueeze(1).to_broadcast([128, NT, 8]), ALU.mult, waits=[t])
                t_cs = _sincos(P, ang, sin_t, cos_t, argt, argi, [t])

                t_x = [None] * NT
                t_hT_free = [None, None]
                t_htok_free = [None, None]
                t_tp_free = [None, None]
                t_qkrot_free = [None, None]
                t_bank_free = [[], [], [], []]
                stA = {}
                stB = {}
                sd = {"qtps_free": None, "dbg_free": None}
                qk_v = u_ps[:, 512:1536].rearrange("p (a d) -> p a d", d=64)
                x1 = qk_v[:, :, 0:8]
                x2 = qk_v[:, :, 8:16]

                def stageA(i):
                    b = i % 2
                    t_x[i] = P.dma("sync", d_x[i % 4], x_res[:, i, :], x_d[i * 128:(i + 1) * 128, :],
                                   waits=[t_x[i - 4]] if i >= 4 else [])
                    t_ss = P.op("scalar", "activation", out=junk[:], in_=x_res[:, i, :], func=AF.Square,
                                accum_out=ss[:, i:i + 1], waits=[t_x[i], sd.get("junk")])
                    sd["junk"] = t_ss
                    t_r = _rstd(P, ss[:, i:i + 1], rstd[:, i:i + 1], D, [t_ss, t_eps], eps_t[:])
                    t_h = P.op("vector", "scalar_tensor_tensor", out=h_tok[b][:], in0=x_res[:, i, :], scalar=rstd[:, i:i + 1],
                               in1=g_bc[:], op0=ALU.mult, op1=ALU.mult, waits=[t_r, t_c, t_htok_free[b]])
                    t_tp = None
                    for c in range(8):
                        t_tp = P.op("tensor", "transpose", out=tp_ps[b][:, c, :], in_=h_tok[b][:, c * 128:(c + 1) * 128],
                                    identity=ident_b[:], waits=[t_h, t_idb, t_tp_free[b]], sig=(c == 7))
                    t_htok_free[b] = t_tp
                    t_hT = P.op("scalar", "activation", out=hT[b][:], in_=tp_ps[b][:], func=AF.Copy, waits=[t_tp, t_hT_free[b]])
                    t_tp_free[b] = t_hT
                    stA[i] = t_hT

                def stageB(i):
                    b = i % 2
                    t_hT = stA[i]
                    qr3 = qk_rot[b][:].rearrange("p (a d) -> p a d", d=64)
                    t_nb = []
                    for nb in range(4):
                        t_u = None
                        for c in range(8):
                            t_u = P.op("tensor", "matmul", u_ps[:, nb * 512:(nb + 1) * 512], lhsT=hT[b][:, c, :],
                                       rhs=w_in_sb[:, c, nb * 512:(nb + 1) * 512], start=(c == 0), stop=(c == 7),
                                       waits=[t_hT, t_wn[nb]] + t_bank_free[nb], sig=(c == 7))
                        t_nb.append(t_u)
                    t_hT_free[b] = t_nb[3]
                    t_up = P.op("scalar", "activation", out=upool[:, i, :], in_=u_ps[:, 0:512], func=AF.Copy, waits=[t_nb[0]])
                    t_bank_free[0] = [t_up]
                    t_uv = P.op("scalar", "activation", out=v_all[:, i, :, 0:128],
                                in_=u_ps[:, 1536:2048].rearrange("p (h v) -> p h v", h=4), func=AF.Copy, waits=[t_nb[3], t_v1])
                    t_bank_free[3] = [t_uv]
                    t_nr = P.op("scalar", "activation", out=qr3[:, :, 16:64], in_=qk_v[:, :, 16:64], func=AF.Copy,
                                waits=[t_nb[1], t_nb[2], t_qkrot_free[b]])
                    cb = cos_t[:, i:i + 1, :].to_broadcast([128, 16, 8])
                    sn = sin_t[:, i:i + 1, :].to_broadcast([128, 16, 8])
                    ta = P.op("vector", "tensor_tensor", rt[0][:], x1, cb, ALU.mult, waits=[t_nb[1], t_nb[2], t_cs] + sd.get("rt_free", []))
                    tb_ = P.op("vector", "tensor_tensor", rt[1][:], x2, sn, ALU.mult)
                    tc_ = P.op("vector", "tensor_tensor", rt[2][:], x2, cb, ALU.mult)
                    td_ = P.op("vector", "tensor_tensor", rt[3][:], x1, sn, ALU.mult)
                    te_ = P.op("vector", "tensor_tensor", qr3[:, :, 0:8], rt[0][:], rt[1][:], ALU.subtract,
                               waits=[ta, tb_, t_qkrot_free[b]])
                    tf_ = P.op("vector", "tensor_tensor", qr3[:, :, 8:16], rt[2][:], rt[3][:], ALU.add, waits=[tc_, td_])
                    t_bank_free[1] = [t_nr, td_]
                    t_bank_free[2] = [t_nr, td_]
                    sd["rt_free"] = [te_, tf_]
                    if stage == 1:
                        tdb = P.op("vector", "tensor_copy", udbg[:], u_ps[:], waits=t_nb + [sd["dbg_free"]])
                        for nb in range(4):
                            t_bank_free[nb] = t_bank_free[nb] + [tdb]
                        sd["dbg_free"] = P.dma("sync", d_o, dbg["u"][i * 128:(i + 1) * 128, :], udbg[:], waits=[tdb])
                    stB[i] = (t_nr, te_, tf_)

                def stageC(i):
                    b = i % 2
                    t_nr, te_, tf_ = stB[i]
                    t_qt = None
                    for j in range(8):
                        t_qt = P.op("tensor", "transpose", out=qt_ps[:, j, :], in_=qk_rot[b][:, j * 128:(j + 1) * 128],
                                    identity=ident_b[:], waits=[t_nr, te_, tf_, sd["qtps_free"]], sig=(j == 7))
                    t_qkrot_free[b] = t_qt
                    t_qT = P.op("scalar", "activation", out=qkT[:, :, i * 128:(i + 1) * 128], in_=qt_ps[:], func=AF.Copy, waits=[t_qt])
                    sd["qtps_free"] = t_qT

                stageA(0)
                for i in range(NT):
                    if i + 1 < NT:
                        stageA(i + 1)
                    stageB(i)
                    if i >= 1:
                        stageC(i - 1)
                stageC(NT - 1)
                t_dbg_free = sd["dbg_free"]
                if stage == 1:
                    P.wait("sync", [t_dbg_free])
                P.run()
            if stage == 1:
                pd = ExitStack()
                with pd:
                    P = Prog(nc, pd)
                    d_o = P.dsem()
                    tmp = sb("tmpdump", [128, 4, S], F32, pd)
                    qk_dst = dbg["qk"].rearrange("p (a s) -> p a s", a=8)
                    t2 = None
                    for hf in range(2):
                        t = P.op("vector", "tensor_copy", tmp[:], qkT[:, hf * 4:(hf + 1) * 4, :], waits=[t2])
                        for a in range(4):
                            t2 = P.dma("sync", d_o, qk_dst[:, hf * 4 + a, :], tmp[:, a, :], waits=[t])
                    P.wait("sync", [t2])
                    P.run()
                return nc
            w_out_sb = sb("w_out_sb", [128, 8, D], BF16, p1)
            mixT = mixT_w
            pb_ = ExitStack()
            with pb_:
                P = Prog(nc, pb_)
                poolm_sb = sb("poolm_sb", [128, 3, 4, 128], BF16, pb_)
                w_pool_sb = sb("w_pool_sb", [128, 4, 128], BF16, pb_)
                pscale = sb("pscale", [128, 4], F32, pb_)
                mixedT = [sb(f"mixedT{i}", [128, 512], BF16, pb_) for i in range(2)]
                mx_ps = [ps(f"mx_ps{i}", [128, 512], F32, pb_) for i in range(2)]
                yp_ps = [ps(f"yp_ps{i}", [128, 512], F32, pb_) for i in range(2)]
                d_c = P.dsem()
                d_w = P.dsem()
                d_wo = P.dsem()
                P.dma("gpsimd", d_w, poolm_sb[:], poolm_d.rearrange("k p g t -> p k g t"))
                t_w = P.dma("gpsimd", d_w, w_pool_sb[:], w_pool_d.rearrange("g c d -> c g d"))
                t_c = P.dma("sync", d_c, pscale[:], pool_scale_d)
                t_wo = P.dma("gpsimd", d_wo, w_out_sb[:], w_out_d.rearrange("(c p) f -> p c f", p=128))
                t_mixed_free = [None, None]
                t_mx_free = [None, None]
                t_yp_free = [None, None]
                n = 0
                for g in range(4):
                    for tb in range(4):
                        b = n % 2
                        n += 1
                        t_mx = None
                        for jj in range(4):
                            j = tb * 4 + jj
                            kind = 0 if j == 0 else 1
                            t_mx = P.op("tensor", "matmul", mx_ps[b][:, jj * 128:(jj + 1) * 128],
                                        lhsT=upool[:, j, g * 128:(g + 1) * 128], rhs=poolm_sb[:, kind, g, :],
                                        start=True, stop=(j == 0), waits=[t_w, t_mx_free[b]], sig=(j == 0 and jj == 3))
                            if j > 0:
                                t_mx = P.op("tensor", "matmul", mx_ps[b][:, jj * 128:(jj + 1) * 128],
                                            lhsT=upool[64:128, j - 1, g * 128:(g + 1) * 128], rhs=poolm_sb[64:128, 2, g, :],
                                            start=False, stop=True, sig=(jj == 3))
                        t_ev = P.op("scalar", "activation", out=mixedT[b][:], in_=mx_ps[b][:], func=AF.Copy,
                                    waits=[t_mx, t_mixed_free[b]])
                        t_mx_free[b] = t_ev
                        t_yp = P.op("tensor", "matmul", yp_ps[b][:], lhsT=w_pool_sb[:, g, :], rhs=mixedT[b][:],
                                    start=True, stop=True, waits=[t_ev, t_yp_free[b]])
                        t_mixed_free[b] = t_yp
                        t_yv = P.op("vector", "tensor_scalar", mixT[:, g, tb * 512:(tb + 1) * 512], yp_ps[b][:],
                                    pscale[:, g:g + 1], None, ALU.mult, waits=[t_yp, t_c])
                        t_yp_free[b] = t_yv
                P.wait("gpsimd", [t_wo])
                P.run()

            pc = ExitStack()
            with pc:
                P = Prog(nc, pc)
                lam_sb = sb("lam_sb", [128, 256], F32, pc)
                lprod = sb("lprod", [128, 2, 64], F32, pc)
                lsum = sb("lsum", [128, 2], F32, pc)
                neglam = sb("neglam", [128, 1], F32, pc)
                gsub = sb("gsub", [128, 128], F32, pc)
                pT = [sb(f"pT{i}", [128, 512], BF16, pc) for i in range(3)]
                o1 = [sb(f"o1_{i}", [128, 4, 128], F32, pc) for i in range(2)]
                osb = [sb(f"osb{i}", [128, 128], F32, pc) for i in range(2)]
                ojunk = sb("ojunk", [128, 128], BF16, pc)
                y_tok = [sb(f"y_tok{i}", [128, 128], BF16, pc) for i in range(8)]
                rl = sb("rl", [128, 64], F32, pc)
                rl2 = sb("rl2", [128, 64], F32, pc)
                oss = sb("oss", [128, 64], F32, pc)
                orstd = sb("orstd", [128, 64], F32, pc)
                s_ps = [ps(f"s_ps{i}", [128, 512], F32, pc) for i in range(2)]
                acc_ps = [[ps(f"acc_ps{i}_{k}", [128, 512], F32, pc) for k in range(2)] for i in range(2)]
                yt_ps = ps("yt_ps", [128, 4, 128], BF16, pc)
                d_c = P.dsem()
                P.dma("sync", d_c, lam_sb[:], lam_d.partition_broadcast(128))
                t_c = P.dma("sync", d_c, gsub[:], subln_g_d.partition_broadcast(128))
                zt = sb("zt", [128, 4096], BF16, pc)
                d_z = P.dsem()
                t_zm = P.op("gpsimd", "memset", zt[:], 0.0)
                t_zd = None
                for r0 in range(0, NROWS + 128, 512):
                    t_zd = P.dma("sync", d_z, xd_d[r0:r0 + 512, :].rearrange("(p s) d -> p (s d)", s=4), zt[:], waits=[t_zm])
                lv = lam_sb[:].rearrange("p (a b d) -> p a b d", a=2, b=2)
                t = P.op("vector", "tensor_tensor", lprod[:], lv[:, :, 0, :], lv[:, :, 1, :], ALU.mult, waits=[t_c])
                t = P.op("vector", "tensor_reduce", lsum[:], lprod[:], AX.X, ALU.add, waits=[t])
                t = P.op("scalar", "activation", out=lsum[:], in_=lsum[:], func=AF.Exp, waits=[t])
                t = P.op("vector", "tensor_tensor", neglam[:], lsum[:, 1:2], lsum[:, 0:1], ALU.subtract, waits=[t])
                t_lam = P.op("vector", "tensor_scalar_add", neglam[:], neglam[:], -LAM_INIT, waits=[t])
                t_gs = P.op("vector", "tensor_scalar_mul", gsub[:], gsub[:], 1.0 - LAM_INIT, waits=[t_c])

                s_ring = Ring(s_ps)
                pT_ring = Ring(pT)
                t_acc_free = [[None, None], [None, None]]
                t_o1_free = [None, None]
                t_osb_free = [None, None]
                t_ytok_free = [None] * 8
                st8 = {"ytps_free": None, "nq": 0}

                rounds = []
                for h in range(4):
                    for qb in range(4):
                        for m in range(2):
                            rounds.append(dict(h=h, qb=qb, m=m, ab=len(rounds) % 2, ob=(h * 4 + qb) % 2,
                                               bank_started=[False, False], t_last=[None] * 4))
                steps = [(r, kt) for r in rounds for kt in range(4 * r["qb"] + 4)]

                def emit_S(r, kt):
                    h, qb, m = r["h"], r["qb"], r["m"]
                    c0 = max(0, kt - 4 * qb)
                    si, sbuf, sfr = s_ring.get()
                    pi, pbuf, pfr = pT_ring.get()
                    t_s = P.op("tensor", "matmul", sbuf[:, c0 * 128:512],
                               lhsT=qkT[m * 64:(m + 1) * 64, 4 + h, kt * 128:(kt + 1) * 128],
                               rhs=qkT[m * 64:(m + 1) * 64, h, qb * 512 + c0 * 128:(qb + 1) * 512],
                               start=True, stop=True, waits=[sfr])
                    t_e = P.op("scalar", "activation", out=pbuf[:, c0 * 128:512], in_=sbuf[:, c0 * 128:512],
                               func=AF.Exp, scale=0.125, waits=[t_s, pfr])
                    s_ring.release(si, t_e)
                    if kt >= 4 * qb:
                        t_e = P.op("vector", "memset", pbuf[64:128, c0 * 128:c0 * 128 + 64], 0.0, waits=[t_e])
                    return (c0, pi, pbuf, t_e)

                def emit_PV(r, kt, sres):
                    h, qb, ab = r["h"], r["qb"], r["ab"]
                    c0, pi, pbuf, t_e = sres
                    t_pv = None
                    for ql in range(c0, 4):
                        bk = ql // 2
                        off = (ql % 2) * 256
                        last = (kt == 4 * qb + ql)
                        t_pv = P.op("tensor", "matmul", acc_ps[ab][bk][:, off:off + 129],
                                    lhsT=pbuf[:, ql * 128:(ql + 1) * 128], rhs=v_all[:, kt, h, 0:129],
                                    start=(not r["bank_started"][bk]), stop=last, skip_group_check=True,
                                    waits=[t_e, t_acc_free[ab][bk]], sig=(last or ql == 3))
                        r["bank_started"][bk] = True
                        if last:
                            r["t_last"][ql] = t_pv
                    pT_ring.release(pi, t_pv)

                def emit_evac(r):
                    h, qb, m, ab, ob = r["h"], r["qb"], r["m"], r["ab"], r["ob"]
                    t_last = r["t_last"]
                    t_ev_bank = [None, None]
                    for ql in range(4):
                        bk = ql // 2
                        off = (ql % 2) * 256
                        acc = acc_ps[ab][bk]
                        if m == 0:
                            ci = (st8["nq"] + ql) % 64
                            t_r = P.op("vector", "reciprocal", rl[:, ci:ci + 1], acc[:, off + 128:off + 129], waits=[t_last[ql], t_last[2 * bk + 1]])
                            t_o = P.op("vector", "tensor_scalar", o1[ob][:, ql, :], acc[:, off:off + 128], rl[:, ci:ci + 1], None,
                                       ALU.mult, waits=[t_r, t_o1_free[ob]])
                            t_ev_bank[bk] = t_o
                        else:
                            nq = st8["nq"]
                            ci = nq % 64
                            yb = nq % 8
                            ob2 = nq % 2
                            st8["nq"] = nq + 1
                            t_r = P.op("vector", "reciprocal", rl2[:, ci:ci + 1], acc[:, off + 128:off + 129], waits=[t_last[ql], t_last[2 * bk + 1]])
                            t_r = P.op("vector", "tensor_tensor", rl2[:, ci:ci + 1], rl2[:, ci:ci + 1], neglam[:], ALU.mult,
                                       waits=[t_r, t_lam])
                            t_o = P.op("vector", "scalar_tensor_tensor", out=osb[ob2][:], in0=acc[:, off:off + 128],
                                       scalar=rl2[:, ci:ci + 1], in1=o1[ob][:, ql, :], op0=ALU.mult, op1=ALU.add,
                                       waits=[t_r, t_osb_free[ob2]])
                            t_ev_bank[bk] = t_o
                            t_ss = P.op("scalar", "activation", out=ojunk[:], in_=osb[ob2][:], func=AF.Square,
                                        accum_out=oss[:, ci:ci + 1], waits=[t_o])
                            t_rs = _rstd(P, oss[:, ci:ci + 1], orstd[:, ci:ci + 1], 128, [t_ss], eps_t[:])
                            t_y = P.op("vector", "scalar_tensor_tensor", out=y_tok[yb][:], in0=osb[ob2][:],
                                       scalar=orstd[:, ci:ci + 1], in1=gsub[:], op0=ALU.mult, op1=ALU.mult,
                                       waits=[t_rs, t_gs, t_ytok_free[yb]])
                            t_osb_free[ob2] = t_y
                            r.setdefault("ty", []).append((ql, yb, t_y))
                            if ql == 3:
                                t_o1_free[ob] = t_o
                    t_acc_free[ab] = list(t_ev_bank)

                def emit_ytrans(r):
                    if r["m"] == 0:
                        return
                    h, qb = r["h"], r["qb"]
                    t_t = None
                    for (ql, yb, t_y) in r["ty"]:
                        t_t = P.op("tensor", "transpose", out=yt_ps[:, ql, :], in_=y_tok[yb][:], identity=ident_b[:],
                                   waits=[t_y, st8["ytps_free"] if ql == 0 else None])
                        t_ytok_free[yb] = t_t
                    t_cp = P.op("scalar", "activation", out=mixT[:, 4 + h, qb * 512:(qb + 1) * 512],
                                in_=yt_ps[:].rearrange("p a b -> p (a b)"), func=AF.Copy, waits=[t_t])
                    st8["ytps_free"] = t_cp

                prev = None
                pend_tr = None
                for j, (r, kt) in enumerate(steps):
                    sres = emit_S(r, kt)
                    if prev is not None:
                        pr_, pkt, psres = prev
                        emit_PV(pr_, pkt, psres)
                        if pkt == 4 * pr_["qb"] + 3:
                            if pend_tr is not None:
                                emit_ytrans(pend_tr)
                            emit_evac(pr_)
                            pend_tr = pr_
                    prev = (r, kt, sres)
                pr_, pkt, psres = prev
                emit_PV(pr_, pkt, psres)
                if pend_tr is not None:
                    emit_ytrans(pend_tr)
                emit_evac(pr_)
                emit_ytrans(pr_)
                P.wait("sync", [t_zd])
                P.run()

            pw = ExitStack()
            with pw:
                P = Prog(nc, pw)
                wo_ps = [ps(f"wo_ps{i}", [128, 512], F32, pw) for i in range(4)]
                d_o = P.dsem()
                t_free = [None] * 4
                n = 0
                t_dump = None
                for i in range(NT):
                    tv = None
                    for hf in range(2):
                        b = n % 4
                        n += 1
                        t_m = None
                        for c in range(8):
                            t_m = P.op("tensor", "matmul", wo_ps[b][:], lhsT=mixT[:, c, i * 128:(i + 1) * 128],
                                       rhs=w_out_sb[:, c, hf * 512:(hf + 1) * 512], start=(c == 0), stop=(c == 7),
                                       waits=[t_free[b]], sig=(c == 7))
                        tv = P.op("vector", "tensor_tensor", x_res[:, i, hf * 512:(hf + 1) * 512], wo_ps[b][:],
                                  x_res[:, i, hf * 512:(hf + 1) * 512], ALU.add, waits=[t_m])
                        t_free[b] = tv
                    if stage == 2:
                        t_dump = P.dma("sync", d_o, dbg["x"][i * 128:(i + 1) * 128, :], x_res[:, i, :], waits=[tv])
                if stage == 2:
                    P.wait("sync", [t_dump])
                P.run()
            if stage == 2:
                return nc
        p2 = ExitStack()
        with p2:
            kcT = sb("kcT", [128, 8, MEM], BF16, p2)
            vc = sb("vc", [128, 2, D], BF16, p2)
            w_cq_sb = sb("w_cq_sb", [128, 8, D], BF16, p2)
            w_co_sb = sb("w_co_sb", [128, 8, D], BF16, p2)
            g2_bc = sb("g2_bc", [128, D], F32, p2)
            pm_ = ExitStack()
            with pm_:
                P = Prog(nc, pm_)
                w_ckv_sb = sb("w_ckv_sb", [128, 8, 2 * D], BF16, pm_)
                mem_sb = sb("mem_sb", [128, 2, D], F32, pm_)
                gm_bc = sb("gm_bc", [128, D], F32, pm_)
                memT = sb("memT", [128, 8, MEM], BF16, pm_)
                mss = sb("mss", [128, 2], F32, pm_)
                mrstd = sb("mrstd", [128, 2], F32, pm_)
                junk = sb("junk2", [128, D], BF16, pm_)
                h_tok = [sb(f"m_tok{i}", [128, D], BF16, pm_) for i in range(2)]
                tp_ps = [ps(f"tp2_ps{i}", [128, 8, 128], BF16, pm_) for i in range(2)]
                gen = Ring([ps(f"gen2a_{i}", [128, 512], F32, pm_) for i in range(4)])
                d_c = P.dsem()
                d_w = P.dsem()
                d_w2 = P.dsem()
                P.dma("sync", d_c, mem_sb[:], mem_d.rearrange("(t p) d -> p t d", p=128))
                P.dma("sync", d_c, g2_bc[:], xattn_g_d.partition_broadcast(128))
                t_c = P.dma("sync", d_c, gm_bc[:], mem_g_d.partition_broadcast(128))
                w_ckv_v = w_ckv_d.rearrange("(c p) f -> p c f", p=128)
                t_wk = P.dma("gpsimd", d_w, w_ckv_sb[:, :, 0:D], w_ckv_v[:, :, 0:D])
                d_wv = P.dsem()
                t_wv = P.dma("gpsimd", d_wv, w_ckv_sb[:, :, D:2 * D], w_ckv_v[:, :, D:2 * D])
                P.dma("gpsimd", d_w2, w_cq_sb[:], w_cq_d.rearrange("(c p) f -> p c f", p=128))
                t_w2 = P.dma("gpsimd", d_w2, w_co_sb[:], w_co_d.rearrange("(c p) f -> p c f", p=128))
                t_mT = []
                for mt in range(2):
                    t_ss = P.op("scalar", "activation", out=junk[:], in_=mem_sb[:, mt, :], func=AF.Square,
                                accum_out=mss[:, mt:mt + 1], waits=[t_c])
                    t_r = _rstd(P, mss[:, mt:mt + 1], mrstd[:, mt:mt + 1], D, [t_ss], eps_t[:])
                    t_h = P.op("vector", "scalar_tensor_tensor", out=h_tok[mt][:], in0=mem_sb[:, mt, :], scalar=mrstd[:, mt:mt + 1],
                               in1=gm_bc[:], op0=ALU.mult, op1=ALU.mult, waits=[t_r, t_c])
                    t_tp = None
                    for c in range(8):
                        t_tp = P.op("tensor", "transpose", out=tp_ps[mt][:, c, :], in_=h_tok[mt][:, c * 128:(c + 1) * 128],
                                    identity=ident_b[:], waits=[t_h], sig=(c == 7))
                    t_mT.append(P.op("scalar", "activation", out=memT[:, :, mt * 128:(mt + 1) * 128], in_=tp_ps[mt][:],
                                     func=AF.Copy, waits=[t_tp]))
                for fch in range(8):
                    bi, buf, fr = gen.get()
                    t_m = None
                    for c in range(8):
                        t_m = P.op("tensor", "matmul", buf[:, 0:MEM], lhsT=w_ckv_sb[:, c, fch * 128:(fch + 1) * 128],
                                   rhs=memT[:, c, :], start=(c == 0), stop=(c == 7), waits=[t_wk, fr] + t_mT, sig=(c == 7))
                    gen.release(bi, P.op("scalar", "activation", out=kcT[:, fch, :], in_=buf[:, 0:MEM], func=AF.Copy, waits=[t_m]))
                for mt in range(2):
                    for hf in range(2):
                        bi, buf, fr = gen.get()
                        t_m = None
                        for c in range(8):
                            t_m = P.op("tensor", "matmul", buf[:], lhsT=memT[:, c, mt * 128:(mt + 1) * 128],
                                       rhs=w_ckv_sb[:, c, D + hf * 512:D + (hf + 1) * 512], start=(c == 0), stop=(c == 7),
                                       waits=[t_wv, fr], sig=(c == 7))
                        gen.release(bi, P.op("vector", "tensor_copy", vc[:, mt, hf * 512:(hf + 1) * 512], buf[:], waits=[t_m]))
                P.wait("gpsimd", [t_w2])
                P.run()

            px = ExitStack()
            with px:
                P = Prog(nc, px)
                h2T = sb("h2T", [128, 8, S], BF16, px)
                qcT = sb("qcT", [128, 8, S], BF16, px)
                xss = sb("xss", [128, NT], F32, px)
                xrstd = sb("xrstd", [128, NT], F32, px)
                junk = sb("junk3", [128, D], BF16, px)
                h_tok = [sb(f"x_tok{i}", [128, D], BF16, px) for i in range(2)]
                pT = [[sb(f"cpT{i}_{k}", [128, 512], BF16, px) for k in range(2)] for i in range(2)]
                rl = [sb(f"crl{i}", [128, 512], F32, px) for i in range(2)]
                tp_ps = [ps(f"tp3_ps{i}", [128, 8, 128], BF16, px) for i in range(2)]
                gen = Ring([ps(f"gen2b_{i}", [128, 512], F32, px) for i in range(6)])
                d_o = P.dsem()
                t_htok_free = [None, None]
                t_tp_free = [None, None]
                t_pT_free = [None, None]
                t_rl_free = [None, None]
                sd2 = {"nh": 0, "dump": None}
                hT_tok = {}
                q_tok = {}

                def stA(tb):
                    t_hT = []
                    for ii in range(4):
                        i = tb * 4 + ii
                        b = i % 2
                        t_ss = P.op("scalar", "activation", out=junk[:], in_=x_res[:, i, :], func=AF.Square,
                                    accum_out=xss[:, i:i + 1])
                        t_r = _rstd(P, xss[:, i:i + 1], xrstd[:, i:i + 1], D, [t_ss], eps_t[:])
                        t_h = P.op("vector", "scalar_tensor_tensor", out=h_tok[b][:], in0=x_res[:, i, :], scalar=xrstd[:, i:i + 1],
                                   in1=g2_bc[:], op0=ALU.mult, op1=ALU.mult, waits=[t_r, t_htok_free[b]])
                        t_tp = None
                        for c in range(8):
                            t_tp = P.op("tensor", "transpose", out=tp_ps[b][:, c, :], in_=h_tok[b][:, c * 128:(c + 1) * 128],
                                        identity=ident_b[:], waits=[t_h, t_tp_free[b]], sig=(c == 7))
                        t_htok_free[b] = t_tp
                        t_cp = P.op("scalar", "activation", out=h2T[:, :, i * 128:(i + 1) * 128], in_=tp_ps[b][:], func=AF.Copy,
                                    waits=[t_tp])
                        t_tp_free[b] = t_cp
                        t_hT.append(t_cp)
                    hT_tok[tb] = t_hT

                def stQ(tb):
                    tsl = slice(tb * 512, (tb + 1) * 512)
                    t_q = []
                    for fch in range(8):
                        bi, buf, fr = gen.get()
                        t_m = None
                        for c in range(8):
                            t_m = P.op("tensor", "matmul", buf[:], lhsT=w_cq_sb[:, c, fch * 128:(fch + 1) * 128],
                                       rhs=h2T[:, c, tsl], start=(c == 0), stop=(c == 7), waits=hT_tok[tb] + [fr], sig=(c == 7))
                        if fch % 2 == 0:
                            t_e = P.op("scalar", "activation", out=qcT[:, fch, tsl], in_=buf[:], func=AF.Copy, waits=[t_m])
                        else:
                            t_e = P.op("vector", "tensor_copy", qcT[:, fch, tsl], buf[:], waits=[t_m])
                        gen.release(bi, t_e)
                        t_q.append(t_e)
                    q_tok[tb] = t_q

                def stS(tb, hh):
                    tsl = slice(tb * 512, (tb + 1) * 512)
                    t_q = q_tok[tb]
                    pb = sd2["nh"] % 2
                    sd2["nh"] += 1
                    t_p = []
                    for mt in range(2):
                        bi, buf, fr = gen.get()
                        t_m = None
                        for j in range(2):
                            t_m = P.op("tensor", "matmul", buf[:], lhsT=kcT[:, 2 * hh + j, mt * 128:(mt + 1) * 128],
                                       rhs=qcT[:, 2 * hh + j, tsl], start=(j == 0), stop=(j == 1),
                                       waits=[t_q[2 * hh], t_q[2 * hh + 1], fr], sig=(j == 1))
                        t_e = P.op("scalar", "activation", out=pT[pb][mt][:], in_=buf[:], func=AF.Exp, scale=1.0 / 16.0,
                                   waits=[t_m, t_pT_free[pb]])
                        gen.release(bi, t_e)
                        t_p.append(t_e)
                    return (pb, t_p)

                def stL(tb, hh, sres):
                    tsl = slice(tb * 512, (tb + 1) * 512)
                    t_q = q_tok[tb]
                    pb, t_p = sres
                    bl, lbuf, fr = gen.get()
                    t_l = None
                    for mt in range(2):
                        t_l = P.op("tensor", "matmul", lbuf[:], lhsT=ones_b[:], rhs=pT[pb][mt][:], start=(mt == 0), stop=(mt == 1),
                                   waits=t_p + [fr], sig=(mt == 1))
                    t_rl = P.op("vector", "reciprocal", rl[pb][:], lbuf[:], waits=[t_l, t_rl_free[pb]])
                    gen.release(bl, t_rl)
                    t_o = None
                    t_m = None
                    for j in range(2):
                        bo, obuf, fr = gen.get()
                        for mt in range(2):
                            t_m = P.op("tensor", "matmul", obuf[:], lhsT=vc[:, mt, (2 * hh + j) * 128:(2 * hh + j + 1) * 128],
                                       rhs=pT[pb][mt][:], start=(mt == 0), stop=(mt == 1), waits=[fr], sig=(mt == 1))
                        t_o = P.op("vector", "tensor_tensor", qcT[:, 2 * hh + j, tsl], obuf[:], rl[pb][:], ALU.mult, waits=[t_m, t_rl])
                        gen.release(bo, t_o)
                        t_q[2 * hh + j] = t_o
                    t_pT_free[pb] = t_m
                    t_rl_free[pb] = t_o

                def stATT(tb):
                    prev = None
                    for hh in range(4):
                        sres = stS(tb, hh)
                        if prev is not None:
                            stL(tb, prev[0], prev[1])
                        prev = (hh, sres)
                    stL(tb, prev[0], prev[1])

                def stO(tb):
                    t_q = q_tok[tb]
                    for ii in range(4):
                        i = tb * 4 + ii
                        tv = None
                        for hf in range(2):
                            bi, buf, fr = gen.get()
                            t_m = None
                            for c in range(8):
                                t_m = P.op("tensor", "matmul", buf[:], lhsT=qcT[:, c, i * 128:(i + 1) * 128],
                                           rhs=w_co_sb[:, c, hf * 512:(hf + 1) * 512], start=(c == 0), stop=(c == 7),
                                           waits=t_q + [fr], sig=(c == 7))
                            tv = P.op("vector", "tensor_tensor", x_res[:, i, hf * 512:(hf + 1) * 512], buf[:],
                                      x_res[:, i, hf * 512:(hf + 1) * 512], ALU.add, waits=[t_m])
                            gen.release(bi, tv)
                        if stage == 3:
                            sd2["dump"] = P.dma("sync", d_o, dbg["x"][i * 128:(i + 1) * 128, :], x_res[:, i, :], waits=[tv])

                stA(0)
                stQ(0)
                stA(1)
                for tb in range(4):
                    stATT(tb)
                    if tb + 1 < 4:
                        stQ(tb + 1)
                    stO(tb)
                    if tb + 2 < 4:
                        stA(tb + 2)
                t_dump = sd2["dump"]
                if stage == 3:
                    P.wait("sync", [t_dump])
                P.run()
            if stage == 3:
                return nc
        yd_d = nc.dram_tensor("yd_scr", [NROWS + 128, D], F32, kind="Internal").ap()
        x2_d = nc.dram_tensor("x2_scr", [S, D], F32, kind="Internal").ap()
        p3 = ExitStack()
        with p3:
            idx = sb("idx", [128, 4, NT], I32, p3)
            gates = sb("gates", [128, 4, NT], F32, p3)
            bgu_sb = sb("bgu_sb", [128, NE * 16], F32, p3)
            wd = [sb(f"wd{i}", [128, 8, D], BF16, p3) for i in range(2)]
            xrb = x_res[:].bitcast(BF16)
            wgu = [xrb[:, sl * 8:(sl + 1) * 8, :] for sl in range(2)]
            d_wg = [Sem(p3.enter_context(nc.semaphore(f"xwg{i}"))) for i in range(2)]
            d_wd = [Sem(p3.enter_context(nc.semaphore(f"xwd{i}"))) for i in range(2)]
            wtok = {}
            idxw = sb("idxw", [128, NE, 8], I32, p3)
            idxb = sb("idxb", [128, NE], I32, p3)
            oh_r = sb("oh_r", [128, NE, NE], F32, p3)
            d_bdw = [Sem(p3.enter_context(nc.semaphore(f"xbd{i}"))) for i in range(2)]
            wgu_rows = w_gu_d.rearrange("e r f -> (e r) f")
            wdn_rows = w_down_d.rearrange("e r f -> (e r) f")

            def issue_wloads(P, e, waits_g=(), waits_d=()):
                sl = e % 2
                t_g = None
                for c in range(8):
                    t_g = P.dma("gpsimd", d_wg[sl], wgu[sl][:, c, :], wgu_rows, waits=list(waits_g) if c == 0 else [],
                                meth="indirect_dma_start", out_offset=None,
                                in_offset=bass.IndirectOffsetOnAxis(ap=idxw[:, e, c:c + 1], axis=0))
                t_d = None
                for c in range(8):
                    t_d = P.dma("gpsimd", d_wd[sl], wd[sl][:, c, :], wdn_rows, waits=list(waits_d) if c == 0 else [],
                                meth="indirect_dma_start", out_offset=None,
                                in_offset=bass.IndirectOffsetOnAxis(ap=idxw[:, e, c:c + 1], axis=0))
                wtok[e] = (t_g, t_d)
            pr = ExitStack()
            with pr:
                P = Prog(nc, pr)
                h3_all = sb("h3_all", [128, NT, D], BF16, pr)
                g3_bc = sb("g3_bc", [128, D], F32, pr)
                wr_sb = sb("wr_sb", [128, 8, NE], F32, pr)
                br_bc = sb("br_bc", [128, NE], F32, pr)
                ltm = sb("ltm", [128, NE, NE], F32, pr)
                tab = sb("tab", [128, 3 * NE], F32, pr)
                iotacp = sb("iotacp", [128, 8], F32, pr)
                cnt = sb("cnt", [128, NE], F32, pr)
                rank = sb("rank", [128, NE], F32, pr)
                base_e = sb("base_e", [128, NE], F32, pr)
                cap_e = sb("cap_e", [128, NE], F32, pr)
                perm = sb("perm", [128, NE], F32, pr)
                idxwf = sb("idxwf", [128, NE, 8], F32, pr)
                tri_f = sb("tri_f", [128, 128], F32, pr)
                trash = sb("trash", [128, 1], F32, pr)
                zrow = sb("zrow", [128, D], F32, pr)
                tri_b = sb("tri_b", [128, 128], BF16, pr)
                bgu_raw = sb("bgu_raw", [128, 4, 128], F32, pr)
                rss = sb("rss", [128, NT], F32, pr)
                rrstd = sb("rrstd", [128, NT], F32, pr)
                junk = sb("junk4", [128, D], BF16, pr)
                h3f = [sb(f"h3f{i}", [128, D], F32, pr) for i in range(2)]
                h3T = [sb(f"h3T{i}", [128, 8, 128], F32, pr) for i in range(2)]
                lg = sb("lg", [128, NT, NE], F32, pr)
                work = sb("work", [128, NT, NE], F32, pr)
                cm1 = h3f[0][:].rearrange("p (a b) -> p a b", b=NE)
                cm2 = h3f[1][:].rearrange("p (a b) -> p a b", b=NE)
                ovff = work
                oh = [sb(f"oh{k}", [128, NT, NE], F32, pr) for k in range(4)]
                mask_f = sb("mask_f", [128, NT, NE], F32, pr)
                mask_b = sb("mask_b", [128, NT * NE], BF16, pr)
                mv = sb("mv", [128, 4, NT], F32, pr)
                evk = sb("evk", [128, 4, NT], F32, pr)
                den = sb("den", [128, NT], F32, pr)
                tot = sb("tot", [128, NT, NE], F32, pr)
                basec = sb("basec", [128, NT, NE], F32, pr)
                pos = sb("rpos", [128, NT, NE], F32, pr)
                dest = sb("rdest", [128, NT, NE], F32, pr)
                tmp3 = sb("tmp3", [128, NT, NE], F32, pr)
                destk = sb("destk", [128, 4, NT], F32, pr)
                posk = sb("posk", [128, 4, NT], F32, pr)
                ovf = sb("ovf", [128, 4, NT], F32, pr)
                tpf = [[ps(f"tpf{i}_{k}", [128, 4, 128], F32, pr) for k in range(2)] for i in range(2)]
                lg_ps = ps("lg_ps", [128, NT, NE], F32, pr)
                tot_ps = ps("tot_ps", [128, NT * NE], F32, pr)
                win_ps = ps("win_ps", [128, NT * NE], F32, pr)
                d_c = P.dsem()
                d_x2 = P.dsem()
                d_sc = [P.dsem() for _ in range(4)]
                P.dma("sync", d_c, g3_bc[:], ffn_g_d.partition_broadcast(128))
                P.dma("sync", d_c, wr_sb[:], w_router_d.rearrange("(c p) e -> p c e", p=128))
                P.dma("sync", d_c, br_bc[:], b_router_d.partition_broadcast(128))
                P.dma("sync", d_c, ltm[:].rearrange("p a b -> p (a b)"), lt_d.partition_broadcast(128))
                P.dma("sync", d_c, tab[:], tab_d.partition_broadcast(128))
                P.dma("sync", d_c, iotacp[:], iotacp_d)
                P.dma("sync", d_c, tri_f[:], tri_d)
                P.dma("sync", d_c, trash[:], trash_d)
                t_c = P.dma("sync", d_c, bgu_raw[:], b_gu_d.rearrange("(a p) f -> p a f", p=128))
                t_tri = P.op("vector", "tensor_copy", tri_b[:], tri_f[:], waits=[t_c])
                t_z = P.op("vector", "memset", zrow[:], 0.0)
                t_x2 = P.dma("sync", d_x2, yd_d[NROWS:NROWS + 128, :], zrow[:], waits=[t_z])
                for a in range(4):
                    tt = P.op("tensor", "transpose", out=tpf[0][0][:, 0, :], in_=bgu_raw[:, a, :], identity=ident_f[:],
                              waits=[t_c] if a == 0 else [tcp])
                    tcp = P.op("vector", "tensor_copy", bgu_sb[:, a * 128:(a + 1) * 128], tpf[0][0][:, 0, :], waits=[tt])
                t_h3f_free = [None, None]
                t_tpf_free = [tcp, None]
                t_h3T_free = [None, None]
                t_x2 = None
                t_lg = None
                for i in range(NT):
                    b = i % 2
                    t_x2 = P.dma("sync", d_x2, x2_d[i * 128:(i + 1) * 128, :], x_res[:, i, :])
                    t_ss = P.op("scalar", "activation", out=junk[:], in_=x_res[:, i, :], func=AF.Square, accum_out=rss[:, i:i + 1])
                    t_r = _rstd(P, rss[:, i:i + 1], rrstd[:, i:i + 1], D, [t_ss], eps_t[:])
                    t_h = P.op("vector", "scalar_tensor_tensor", out=h3f[b][:], in0=x_res[:, i, :], scalar=rrstd[:, i:i + 1],
                               in1=g3_bc[:], op0=ALU.mult, op1=ALU.mult, waits=[t_r, t_c, t_h3f_free[b]])
                    t_hb = P.op("scalar", "activation", out=h3_all[:, i, :], in_=h3f[b][:], func=AF.Copy, waits=[t_h])
                    t_tp = None
                    for c in range(8):
                        t_tp = P.op("tensor", "transpose", out=tpf[b][c // 4][:, c % 4, :], in_=h3f[b][:, c * 128:(c + 1) * 128],
                                    identity=ident_f[:], waits=[t_h, t_tpf_free[b]], sig=(c == 7))
                    t_cp0 = P.op("vector", "tensor_copy", h3T[b][:, 0:4, :], tpf[b][0][:], waits=[t_tp, t_h3T_free[b]])
                    t_cp1 = P.op("vector", "tensor_copy", h3T[b][:, 4:8, :], tpf[b][1][:], waits=[t_tp])
                    t_tpf_free[b] = t_cp1
                    for c in range(8):
                        t_lg = P.op("tensor", "matmul", lg_ps[:, i, :], lhsT=h3T[b][:, c, :], rhs=wr_sb[:, c, :],
                                    start=(c == 0), stop=(c == 7), waits=[t_cp0, t_cp1], sig=(c == 7))
                    t_h3T_free[b] = t_lg
                    t_h3f_free[b] = t_lg
                    P.wait("vector", [t_hb])
                t_xres_free = [t_x2, t_ss, t_h]
                t = P.op("vector", "tensor_tensor", lg[:], lg_ps[:], br_bc[:].unsqueeze(1).to_broadcast([128, NT, NE]), ALU.add,
                         waits=[t_lg, t_c])
                t = P.op("vector", "tensor_copy", work[:], lg[:], waits=[t])
                for k in range(4):
                    t = P.op("vector", "tensor_reduce", mv[:, k, :], work[:], AX.X, ALU.max, waits=[t])
                    t = P.op("vector", "tensor_tensor", oh[k][:], work[:], mv[:, k, :].unsqueeze(2).to_broadcast([128, NT, NE]),
                             ALU.is_equal, waits=[t])
                    t = P.op("vector", "scalar_tensor_tensor", out=work[:], in0=oh[k][:], scalar=-1e30, in1=work[:],
                             op0=ALU.mult, op1=ALU.add, waits=[t])
                t = P.op("vector", "tensor_tensor", mask_f[:], oh[0][:], oh[1][:], ALU.add, waits=[t])
                t = P.op("vector", "tensor_tensor", mask_f[:], mask_f[:], oh[2][:], ALU.add, waits=[t])
                t = P.op("vector", "tensor_tensor", mask_f[:], mask_f[:], oh[3][:], ALU.add, waits=[t])
                t_mb = P.op("vector", "tensor_copy", mask_b[:], mask_f[:].rearrange("p a b -> p (a b)"), waits=[t])
                t = P.op("vector", "tensor_tensor", evk[:], mv[:], mv[:, 0:1, :].to_broadcast([128, 4, NT]), ALU.subtract, waits=[t_mb])
                t = P.op("scalar", "activation", out=evk[:], in_=evk[:], func=AF.Exp, waits=[t])
                t = P.op("vector", "tensor_reduce", den[:], evk[:].rearrange("p k t -> p t k"), AX.X, ALU.add, waits=[t])
                t = P.op("vector", "reciprocal", den[:], den[:], waits=[t])
                t_g = P.op("vector", "tensor_tensor", gates[:], evk[:], den[:].unsqueeze(1).to_broadcast([128, 4, NT]), ALU.mult, waits=[t])
                t_tot = P.op("tensor", "matmul", tot_ps[:], lhsT=ones_b[:], rhs=mask_b[:], start=True, stop=True, waits=[t_mb])
                t_win = P.op("tensor", "matmul", win_ps[:], lhsT=tri_b[:], rhs=mask_b[:], start=True, stop=True, waits=[t_tri])
                t = P.op("vector", "tensor_copy", tot[:], tot_ps[:].rearrange("p (a b) -> p a b", b=NE), waits=[t_tot])
                t = P.op("vector", "memset", basec[:, 0, :], 0.0, waits=[t])
                for i in range(1, NT):
                    t = P.op("vector", "tensor_tensor", basec[:, i, :], basec[:, i - 1, :], tot[:, i - 1, :], ALU.add, waits=[t])
                t = P.op("vector", "tensor_tensor", pos[:], win_ps[:].rearrange("p (a b) -> p a b", b=NE), basec[:], ALU.add, waits=[t, t_win])
                t = P.op("vector", "tensor_tensor", cnt[:], basec[:, NT - 1, :], tot[:, NT - 1, :], ALU.add, waits=[t])
                cA = cnt[:].unsqueeze(2).to_broadcast([128, NE, NE])
                cB = cnt[:].unsqueeze(1).to_broadcast([128, NE, NE])
                t = P.op("vector", "tensor_tensor", cm1, cB, cA, ALU.is_gt, waits=[t])
                t = P.op("vector", "tensor_tensor", cm2, cB, cA, ALU.is_equal, waits=[t])
                t = P.op("vector", "tensor_tensor", cm2, cm2, ltm[:], ALU.mult, waits=[t, t_c])
                t = P.op("vector", "tensor_tensor", cm1, cm1, cm2, ALU.add, waits=[t])
                t = P.op("vector", "tensor_reduce", rank[:], cm1, AX.X, ALU.add, waits=[t])
                iota_j = tab[:, 0:NE].unsqueeze(1).to_broadcast([128, NE, NE])
                t_ohr = P.op("vector", "tensor_tensor", oh_r[:], rank[:].unsqueeze(2).to_broadcast([128, NE, NE]), iota_j, ALU.is_equal, waits=[t])
                t = P.op("vector", "tensor_tensor", cm1, oh_r[:], tab[:, NE:2 * NE].unsqueeze(1).to_broadcast([128, NE, NE]), ALU.mult, waits=[t_ohr])
                t = P.op("vector", "tensor_reduce", base_e[:], cm1, AX.X, ALU.add, waits=[t])
                t = P.op("vector", "tensor_tensor", cm2, oh_r[:], tab[:, 2 * NE:3 * NE].unsqueeze(1).to_broadcast([128, NE, NE]), ALU.mult, waits=[t])
                t = P.op("vector", "tensor_reduce", cap_e[:], cm2, AX.X, ALU.add, waits=[t])
                t = P.op("vector", "tensor_tensor", cm1, oh_r[:].rearrange("p e j -> p j e"), iota_j, ALU.mult, waits=[t])
                t = P.op("vector", "tensor_reduce", perm[:], cm1, AX.X, ALU.add, waits=[t])
                t_ib = P.op("vector", "tensor_copy", idxb[:], perm[:], waits=[t])
                t = P.op("vector", "tensor_scalar_mul", perm[:], perm[:], 1024.0, waits=[t_ib])
                t = P.op("vector", "tensor_tensor", idxwf[:], perm[:].unsqueeze(2).to_broadcast([128, NE, 8]),
                         iotacp[:].unsqueeze(1).to_broadcast([128, NE, 8]), ALU.add, waits=[t])
                t_iw = P.op("vector", "tensor_copy", idxw[:], idxwf[:], waits=[t])
                t = P.op("vector", "tensor_tensor", dest[:], pos[:], base_e[:].unsqueeze(1).to_broadcast([128, NT, NE]), ALU.add, waits=[t_iw])
                t = P.op("vector", "tensor_tensor", ovff[:], pos[:], cap_e[:].unsqueeze(1).to_broadcast([128, NT, NE]), ALU.is_ge, waits=[t])
                for k in range(4):
                    t = P.op("vector", "tensor_tensor", tmp3[:], oh[k][:], dest[:], ALU.mult, waits=[t])
                    t = P.op("vector", "tensor_reduce", destk[:, k, :], tmp3[:], AX.X, ALU.add, waits=[t])
                    t = P.op("vector", "tensor_tensor", tmp3[:], oh[k][:], ovff[:], ALU.mult, waits=[t])
                    t = P.op("vector", "tensor_reduce", ovf[:, k, :], tmp3[:], AX.X, ALU.add, waits=[t])
                t = P.op("vector", "tensor_scalar", posk[:], ovf[:], trash[:, 0:1], None, ALU.mult, waits=[t, t_c])
                t = P.op("vector", "tensor_scalar", ovf[:], ovf[:], -1.0, 1.0, ALU.mult, ALU.add, waits=[t])
                t = P.op("vector", "tensor_tensor", destk[:], destk[:], ovf[:], ALU.mult, waits=[t])
                t = P.op("vector", "tensor_tensor", destk[:], destk[:], posk[:], ALU.add, waits=[t])
                t_idx = P.op("vector", "tensor_copy", idx[:], destk[:], waits=[t])
                t = P.op("vector", "tensor_tensor", gates[:], gates[:], ovf[:], ALU.mult, waits=[t_idx, t_g])
                n = 0
                t_sc = [None] * 4
                for i in range(NT):
                    for k in range(4):
                        t_sc[n % 4] = P.dma("gpsimd", d_sc[n % 4], xd_d[:, :], h3_all[:, i, :], waits=[t_idx, t_sc[n % 4]],
                                            meth="indirect_dma_start",
                                            out_offset=bass.IndirectOffsetOnAxis(ap=idx[:, k, i:i + 1], axis=0), in_offset=None)
                        n += 1
                P.wait("gpsimd", t_sc)
                P.wait("sync", [t_x2])
                P.wait("vector", [t])
                P.run()
            if stage == 4:
                pdg = ExitStack()
                with pdg:
                    P = Prog(nc, pdg)
                    d_o = P.dsem()
                    idxf = sb("idxf", [128, 4 * NT], F32, pdg)
                    t = P.op("vector", "tensor_copy", idxf[:], idx[:].rearrange("p a b -> p (a b)"))
                    P.dma("sync", d_o, dbg["x"][0:128, 0:64], idxf[:], waits=[t])
                    t2 = P.dma("sync", d_o, dbg["x"][128:256, 0:64], gates[:].rearrange("p a b -> p (a b)"))
                    P.wait("sync", [t2])
                    P.run()
                return nc

            pe = ExitStack()
            with pe:
                P = Prog(nc, pe)
                xe_tok = [sb(f"xe_tok{i}", [128, NST, D], BF16, pe) for i in range(2)]
                xeT = [sb(f"xeT{i}", [128, 8, CAP], BF16, pe) for i in range(2)]
                hbT = [sb(f"hbT{i}", [128, 8, CAP], BF16, pe) for i in range(2)]
                ye_ring = Ring([sb(f"ye{i}", [128, D], F32, pe) for i in range(3)])
                bd = [sb(f"bd{i}", [128, D], F32, pe) for i in range(2)]
                bsel = [sb(f"bsel{i}", [128, 16], F32, pe) for i in range(2)]
                tmpb = sb("tmpb", [128, NE, 16], F32, pe)
                g1 = [sb(f"g1_{i}", [128, CAP], F32, pe) for i in range(2)]
                sg = [sb(f"sg_{i}", [128, CAP], F32, pe) for i in range(2)]
                u1 = [sb(f"u1_{i}", [128, CAP], F32, pe) for i in range(2)]
                tpx = [ps(f"tpx{i}", [128, 8, 128], BF16, pe) for i in range(2)]
                gu_ring = Ring([ps(f"gu_ps{i}", [128, 512], F32, pe) for i in range(4)])
                dn_ring = Ring([ps(f"dn_ps{i}", [128, 512], F32, pe) for i in range(2)])
                d_xe = [P.dsem() for _ in range(2)]
                d_y = [P.dsem() for _ in range(3)]
                t_wgu_free = [None, None]
                t_wd_free = [None, None]
                t_xe_free = [None, None]
                t_bd_free = [None, None]
                t_xeT_free = [None, None]
                t_hbT_free = [None, None]
                t_tpx_free = [None, None]
                t_g1_free = [None, None]
                t_sg_free = [None, None]
                t_u1_free = [None, None]
                t_bsel_free = [None, None]
                ntp = 0
                nsw = 0
                loads = {}
                bgu3 = bgu_sb[:].rearrange("p (e c) -> p e c", c=16)
                t_tmpb_free = None

                def issue_loads(e):
                    sl = e % 2
                    nt_ = ITEM_TILES[e]
                    if e not in wtok:
                        issue_wloads(P, e, waits_g=[t_wgu_free[sl]], waits_d=[t_wd_free[sl]])
                    t_g, t_d = wtok[e]
                    t_b = P.dma("gpsimd", d_bdw[sl], bd[sl][:], b_down_d[:, :], waits=[t_bd_free[sl]],
                                meth="indirect_dma_start", out_offset=None,
                                in_offset=bass.IndirectOffsetOnAxis(ap=idxb[:, e:e + 1], axis=0))
                    t_x = P.dma("sync", d_xe[sl], xe_tok[sl][:, 0:nt_, :],
                                xd_d[ITEM_BASE[e]:ITEM_BASE[e] + nt_ * 128, :].rearrange("(s p) d -> p s d", p=128),
                                waits=[t_xe_free[sl]])
                    loads[e] = (t_g, t_d, t_x, t_b)

                issue_loads(0)
                issue_loads(1)
                t_yd = []
                for e in range(NE):
                    sl = e % 2
                    cap = PROFILE[e]
                    nt_ = ITEM_TILES[e]
                    t_g, t_d, t_x, t_b = loads[e]
                    t_tb = P.op("vector", "tensor_tensor", tmpb[:], bgu3, oh_r[:, :, e:e + 1].to_broadcast([128, NE, 16]), ALU.mult,
                                waits=[t_tmpb_free])
                    t_bs = P.op("vector", "tensor_reduce", bsel[sl][:], tmpb[:].rearrange("p e c -> p c e"), AX.X, ALU.add,
                                waits=[t_tb, t_bsel_free[sl]])
                    t_tmpb_free = t_bs
                    t_xT = []
                    t_tp = None
                    for st in range(nt_):
                        tb_ = ntp % 2
                        ntp += 1
                        for c in range(8):
                            t_tp = P.op("tensor", "transpose", out=tpx[tb_][:, c, :], in_=xe_tok[sl][:, st, c * 128:(c + 1) * 128],
                                        identity=ident_b[:], waits=[t_x, t_tpx_free[tb_]], sig=(c == 7))
                        t_cp = P.op("scalar", "activation", out=xeT[sl][:, :, st * 128:(st + 1) * 128], in_=tpx[tb_][:], func=AF.Copy,
                                    waits=[t_tp, t_xeT_free[sl]])
                        t_tpx_free[tb_] = t_cp
                        t_xT.append(t_cp)
                    t_xe_free[sl] = t_tp
                    t_hb = None
                    t_gu_last = None
                    for fc in range(8):
                        toks = []
                        bufs = []
                        for half in range(2):
                            col = half * D + fc * 128
                            bi, buf, fr = gu_ring.get()
                            t_m = None
                            for c in range(8):
                                t_m = P.op("tensor", "matmul", buf[:, 0:cap], lhsT=wgu[sl][:, c, col:col + 128], rhs=xeT[sl][:, c, 0:cap],
                                           start=(c == 0), stop=(c == 7), waits=t_xT + [t_g, fr], sig=(c == 7))
                            toks.append(t_m)
                            bufs.append((bi, buf))
                        t_gu_last = toks[1]
                        w = nsw % 2
                        nsw += 1
                        bgc = bsel[sl][:, fc:fc + 1]
                        buc = bsel[sl][:, 8 + fc:8 + fc + 1]
                        t1 = P.op("vector", "tensor_scalar", g1[w][:, 0:cap], bufs[0][1][:, 0:cap], bgc, SWIGLU_LIMIT, ALU.add, ALU.min,
                                  waits=[toks[0], t_g1_free[w], t_bs])
                        gu_ring.release(bufs[0][0], t1)
                        t2 = P.op("scalar", "activation", out=sg[w][:, 0:cap], in_=g1[w][:, 0:cap], func=AF.Sigmoid, scale=SWIGLU_ALPHA,
                                  waits=[t1, t_sg_free[w]])
                        t3 = P.op("vector", "tensor_scalar", u1[w][:, 0:cap], bufs[1][1][:, 0:cap], buc, SWIGLU_LIMIT, ALU.add, ALU.min,
                                  waits=[toks[1], t_u1_free[w]])
                        gu_ring.release(bufs[1][0], t3)
                        t4 = P.op("vector", "tensor_scalar", u1[w][:, 0:cap], u1[w][:, 0:cap], -SWIGLU_LIMIT, 1.0, ALU.max, ALU.add, waits=[t3])
                        t5 = P.op("vector", "tensor_tensor", g1[w][:, 0:cap], g1[w][:, 0:cap], sg[w][:, 0:cap], ALU.mult, waits=[t2])
                        t_sg_free[w] = t5
                        t_hb = P.op("vector", "tensor_tensor", hbT[sl][:, fc, 0:cap], g1[w][:, 0:cap], u1[w][:, 0:cap], ALU.mult,
                                    waits=[t5, t4, t_hbT_free[sl]])
                        t_g1_free[w] = t_hb
                        t_u1_free[w] = t_hb
                    t_wgu_free[sl] = t_gu_last
                    t_xeT_free[sl] = t_gu_last
                    t_bsel_free[sl] = t_hb
                    t_ev = None
                    t_dn = None
                    for st in range(nt_):
                        m_ = min(128, cap - st * 128)
                        yi, ybuf, yfr = ye_ring.get()
                        for hf in range(2):
                            bi, buf, fr = dn_ring.get()
                            for fc in range(8):
                                t_dn = P.op("tensor", "matmul", buf[0:m_, :], lhsT=hbT[sl][:, fc, st * 128:st * 128 + m_],
                                            rhs=wd[sl][:, fc, hf * 512:(hf + 1) * 512], start=(fc == 0), stop=(fc == 7),
                                            waits=[t_hb, t_d, fr], sig=(fc == 7))
                            t_ev = P.op("vector", "tensor_tensor", ybuf[0:m_, hf * 512:(hf + 1) * 512], buf[0:m_, :],
                                        bd[sl][0:m_, hf * 512:(hf + 1) * 512], ALU.add, waits=[t_dn, t_b, yfr])
                            dn_ring.release(bi, t_ev)
                        r0 = ITEM_BASE[e] + st * 128
                        t_st = P.dma("sync", d_y[yi], yd_d[r0:r0 + m_, :], ybuf[0:m_, :], waits=[t_ev])
                        ye_ring.release(yi, t_st)
                        t_yd.append(t_st)
                    t_wd_free[sl] = t_dn
                    t_hbT_free[sl] = t_dn
                    t_bd_free[sl] = t_ev
                    if e + 2 < NE:
                        issue_loads(e + 2)
                P.wait("sync", t_yd[-3:])
                P.run()

            pf = ExitStack()
            with pf:
                P = Prog(nc, pf)
                gf_bc = sb("gf_bc", [128, D], F32, pf)
                NB = 4
                yk = [[x_res[:, 4 * i + k, :] for k in range(4)] for i in range(NB)]
                xa = [sb(f"xa{i}", [128, D], F32, pf)[:] for i in range(NB)]
                ot = [sb(f"ot{i}", [128, D], F32, pf) for i in range(2)]
                junk = sb("junk5", [128, D], BF16, pf)
                fss = sb("fss", [128, NT], F32, pf)
                frstd = sb("frstd", [128, NT], F32, pf)
                d_c = P.dsem()
                d_g = [[P.dsem() for k in range(4)] for i in range(NB)]
                d_xa = [P.dsem() for _ in range(NB)]
                d_out = [P.dsem() for _ in range(2)]
                t_c = P.dma("sync", d_c, gf_bc[:], final_g_d.partition_broadcast(128))
                t_yk_free = [None] * NB
                t_xa_free = [None] * NB
                t_ot_free = [None, None]
                t_outs = [None, None]
                ld = {}

                def stLoad(i):
                    b = i % NB
                    t_x = P.dma("sync", d_xa[b], xa[b], x2_d[i * 128:(i + 1) * 128, :], waits=[t_xa_free[b]])
                    t_gk = []
                    for k in range(4):
                        t_gk.append(P.dma("gpsimd", d_g[b][k], yk[b][k], yd_d[:, :], waits=[t_yk_free[b]],
                                          meth="indirect_dma_start", out_offset=None,
                                          in_offset=bass.IndirectOffsetOnAxis(ap=idx[:, k, i:i + 1], axis=0)))
                    ld[i] = (t_x, t_gk)

                def stComb(i):
                    b = i % NB
                    ob = i % 2
                    t_x, t_gk = ld[i]
                    t = t_x
                    for k in range(4):
                        t = P.op("vector", "scalar_tensor_tensor", out=xa[b], in0=yk[b][k], scalar=gates[:, k, i:i + 1],
                                 in1=xa[b], op0=ALU.mult, op1=ALU.add, waits=[t, t_gk[k]])
                    t_yk_free[b] = t
                    t_ss = P.op("scalar", "activation", out=junk[:], in_=xa[b], func=AF.Square, accum_out=fss[:, i:i + 1], waits=[t])
                    t_r = _rstd(P, fss[:, i:i + 1], frstd[:, i:i + 1], D, [t_ss], eps_t[:])
                    t_o = P.op("vector", "scalar_tensor_tensor", out=ot[ob][:], in0=xa[b], scalar=frstd[:, i:i + 1], in1=gf_bc[:],
                               op0=ALU.mult, op1=ALU.mult, waits=[t_r, t_c, t_ot_free[ob]])
                    t_xa_free[b] = t_o
                    t_outs[ob] = P.dma("sync", d_out[ob], out_d[i * 128:(i + 1) * 128, :], ot[ob][:], waits=[t_o])
                    t_ot_free[ob] = t_outs[ob]

                for i in range(NB - 1):
                    stLoad(i)
                for i in range(NT):
                    if i + NB - 1 < NT:
                        stLoad(i + NB - 1)
                    stComb(i)
                P.wait("sync", t_outs)
                P.run()
    return nc


def host_consts():
    c = {}
    c["c_ident"] = np.eye(128, dtype=np.float32)
    c["c_invf"] = (500000.0 ** (-np.arange(0, 16, 2, dtype=np.float32) / 16)).astype(np.float32).reshape(1, 8)
    pm = np.zeros((3, 128, 4, 128), np.float32)
    wins = (2, 4, 8, 16)
    for g, w in enumerate(wins):
        for t in range(128):
            lo = max(t + 1 - w, 0)
            cnt = t + 1 - lo
            pm[0, lo:t + 1, g, t] = 1.0 / cnt
            pm[0, t, g, t] -= 1.0
            for tp in range(t + 1 - w, t + 1):
                if tp >= 0:
                    pm[1, tp, g, t] = 1.0 / w
                else:
                    pm[2, 128 + tp, g, t] = 1.0 / w
            pm[1, t, g, t] -= 1.0
    c["c_poolm"] = pm
    c["c_tri"] = np.triu(np.ones((128, 128), np.float32), 1)
    c["c_eoff"] = (np.arange(NE, dtype=np.float32) * CAP).reshape(1, NE)
    c["c_trash"] = (NROWS + np.arange(128, dtype=np.float32)).reshape(128, 1)
    c["c_lt"] = np.tril(np.ones((NE, NE), np.float32), -1).reshape(1, NE * NE)
    c["c_tab"] = np.concatenate([np.arange(NE, dtype=np.float32), np.asarray(ITEM_BASE, np.float32),
                                 np.asarray(PROFILE, np.float32)]).reshape(1, 3 * NE)
    c["c_iotacp"] = (np.arange(8, dtype=np.float32)[None, :] * 128 + np.arange(128, dtype=np.float32)[:, None])
    return c


def make_in_maps(inputs):
    f = lambda a: np.ascontiguousarray(np.asarray(a))
    shared = {
        "attn_norm_g": f(inputs["attn_norm_g"]).reshape(1, D),
        "w_in": f(inputs["w_in"][0]),
        "w_pool": f(inputs["w_pool"][0]),
        "pool_scale": f(np.asarray(inputs["pool_scale"][0]).reshape(4, 128).T),
        "lam4": f(np.stack([np.asarray(inputs[k][0]) for k in ("lambda_q1", "lambda_k1", "lambda_q2", "lambda_k2")])).reshape(1, 256),
        "subln_g": f(inputs["subln_g"]).reshape(1, 128),
        "w_out": f(inputs["w_out"][0]),
        "xattn_norm_g": f(inputs["xattn_norm_g"]).reshape(1, D),
        "mem_norm_g": f(inputs["mem_norm_g"]).reshape(1, D),
        "w_cq": f(inputs["w_cq"][0]),
        "w_ckv": f(inputs["w_ckv"][0]),
        "w_co": f(inputs["w_co"][0]),
        "ffn_norm_g": f(inputs["ffn_norm_g"]).reshape(1, D),
        "w_router": f(inputs["w_router"][0]),
        "b_router": f(inputs["b_router"]).reshape(1, NE),
        "w_gu": f(inputs["w_gu"][0]),
        "b_gu": f(inputs["b_gu"][0]).reshape(NE * 16, 128),
        "w_down": f(inputs["w_down"][0]),
        "b_down": f(inputs["b_down"][0]),
        "final_norm_g": f(inputs["final_norm_g"]).reshape(1, D),
    }
    shared.update(host_consts())
    maps = []
    for b in range(NCORES):
        m = dict(shared)
        m["x"] = f(inputs["x"][b])
        m["pos"] = f(np.asarray(inputs["positions"][b]).astype(np.int32).reshape(NT, 128).T)
        m["mem"] = f(inputs["mem"][b])
        maps.append(m)
    return maps


def kernel(**inputs):
    nc = build()
    maps = make_in_maps(inputs)
    res = run_bass_kernel_spmd(nc, maps, core_ids=list(range(NCORES)))
    return np.stack([r["out"] for r in res.results], axis=0).astype(np.float32)
```

```python
from contextlib import ExitStack
import math
import numpy as np
import concourse.bass as bass
import concourse.mybir as mybir
from concourse.bass_utils import run_bass_kernel_spmd

F32 = mybir.dt.float32
BF16 = mybir.dt.bfloat16
I32 = mybir.dt.int32
AF = mybir.ActivationFunctionType
ALU = mybir.AluOpType
AX = mybir.AxisListType

NCORES = 8
S = 2048
D = 1024
NT = S // 128
MEM = 256
NE = 32
CAP = 512
NST = CAP // 128
PROFILE = [512] * 5 + [448] * 7 + [384] * 6 + [320] * 9 + [256] * 5
ITEM_TILES = [(c + 127) // 128 for c in PROFILE]
ITEM_BASE = [sum(ITEM_TILES[:j]) * 128 for j in range(NE)]
NROWS = sum(ITEM_TILES) * 128
EPS = 1e-5
LAM_INIT = 0.8 - 0.6 * math.exp(0.0)
TWO_PI = 2.0 * math.pi
SWIGLU_ALPHA = 1.702
SWIGLU_LIMIT = 7.0


class Sem:
    def __init__(self, h):
        self.h = h
        self.n = 0


class Prog:
    ENGS = ("sync", "scalar", "vector", "gpsimd", "tensor")
    _uid = 0

    def __init__(self, nc, es):
        self.nc = nc
        self.es = es
        Prog._uid += 1
        self.tag = f"p{Prog._uid}"
        self.q = {e: [] for e in self.ENGS}
        self.esem = {}
        for e in ("scalar", "vector", "gpsimd", "tensor"):
            self.esem[e] = Sem(es.enter_context(nc.semaphore(f"{self.tag}_{e}")))
        self.waited = {e: {} for e in self.ENGS}
        self.nds = 0

    def dsem(self):
        self.nds += 1
        return Sem(self.es.enter_context(self.nc.semaphore(f"{self.tag}_d{self.nds}")))

    def _filter(self, eng, waits):
        out = []
        w = self.waited[eng]
        for tok in waits:
            if tok is None:
                continue
            s, v = tok
            if w.get(id(s), 0) >= v:
                continue
            w[id(s)] = v
            out.append((s, v))
        return out

    def op(self, eng, meth, *args, waits=(), sig=True, **kw):
        ws = self._filter(eng, waits)
        sem = self.esem[eng]
        tok = None
        if sig:
            sem.n += 1
            tok = (sem, sem.n)

        def run(e):
            for (s, v) in ws:
                e.wait_ge(s.h, v)
            inst = getattr(e, meth)(*args, **kw)
            if sig:
                inst.then_inc(sem.h, 1)
        self.q[eng].append(run)
        return tok

    def dma(self, eng, ds, out, in_, waits=(), meth="dma_start", **kw):
        ws = self._filter(eng, waits)
        ds.n += 16
        tok = (ds, ds.n)

        def run(e):
            for (s, v) in ws:
                e.wait_ge(s.h, v)
            getattr(e, meth)(out=out, in_=in_, **kw).then_inc(ds.h, 16)
        self.q[eng].append(run)
        return tok

    def wait(self, eng, waits):
        ws = self._filter(eng, waits)

        def run(e):
            for (s, v) in ws:
                e.wait_ge(s.h, v)
        self.q[eng].append(run)

    def run(self):
        q = self.q
        with self.nc.Block(no_gpsimd_drain=True) as blk:
            @blk.sync
            def _(e):
                for f in q["sync"]:
                    f(e)

            @blk.scalar
            def _(e):
                for f in q["scalar"]:
                    f(e)

            @blk.vector
            def _(e):
                for f in q["vector"]:
                    f(e)

            @blk.gpsimd
            def _(e):
                for f in q["gpsimd"]:
                    f(e)

            @blk.tensor
            def _(e):
                for f in q["tensor"]:
                    f(e)


class Ring:
    def __init__(self, bufs):
        self.bufs = bufs
        self.free = [None] * len(bufs)
        self.n = 0

    def get(self):
        i = self.n % len(self.bufs)
        self.n += 1
        return i, self.bufs[i], self.free[i]

    def release(self, i, tok):
        self.free[i] = tok


def _rstd(P, ss_ap, out_ap, n, waits, eps_t):
    t = P.op("scalar", "activation", out=out_ap, in_=ss_ap, func=AF.Ln, bias=eps_t, scale=1.0 / n, waits=waits)
    return P.op("scalar", "activation", out=out_ap, in_=out_ap, func=AF.Exp, scale=-0.5, waits=[t])


def _sincos(P, ang, out_sin, out_cos, tmpf, tmpi, waits):
    prev = []
    for (shift, dst) in ((0.0, out_sin), (0.5 * math.pi, out_cos)):
        t = P.op("vector", "tensor_scalar", tmpf[:], ang[:], shift, 1.0 / TWO_PI, ALU.add, ALU.mult, waits=list(waits) + prev)
        t = P.op("vector", "tensor_copy", tmpi[:], tmpf[:], waits=[t])
        t = P.op("vector", "tensor_copy", tmpf[:], tmpi[:], waits=[t])
        t = P.op("vector", "scalar_tensor_tensor", out=tmpf[:], in0=tmpf[:], scalar=-TWO_PI, in1=ang[:],
                 op0=ALU.mult, op1=ALU.add, waits=[t])
        t = P.op("vector", "tensor_scalar", tmpf[:], tmpf[:], shift, -math.pi, ALU.add, ALU.max, waits=[t])
        t = P.op("vector", "tensor_scalar_min", tmpf[:], tmpf[:], math.pi, waits=[t])
        t = P.op("scalar", "activation", out=dst[:], in_=tmpf[:], func=AF.Sin, waits=[t])
        prev = [t]
    return prev[0]


def build(stage=99):
    nc = bass.Bass("TRN2", target_bir_lowering=False)

    def din(name, shape, dt=F32):
        return nc.dram_tensor(name, list(shape), dt, kind="ExternalInput").ap()

    x_d = din("x", [S, D])
    pos_d = din("pos", [128, NT], I32)
    mem_d = din("mem", [MEM, D])
    attn_g_d = din("attn_norm_g", [1, D])
    w_in_d = din("w_in", [D, 2048])
    w_pool_d = din("w_pool", [4, 128, 128])
    pool_scale_d = din("pool_scale", [128, 4])
    lam_d = din("lam4", [1, 256])
    subln_g_d = din("subln_g", [1, 128])
    w_out_d = din("w_out", [D, D])
    xattn_g_d = din("xattn_norm_g", [1, D])
    mem_g_d = din("mem_norm_g", [1, D])
    w_cq_d = din("w_cq", [D, D])
    w_ckv_d = din("w_ckv", [D, 2 * D])
    w_co_d = din("w_co", [D, D])
    ffn_g_d = din("ffn_norm_g", [1, D])
    w_router_d = din("w_router", [D, NE])
    b_router_d = din("b_router", [1, NE])
    w_gu_d = din("w_gu", [NE, D, 2 * D])
    b_gu_d = din("b_gu", [NE * 16, 128])
    w_down_d = din("w_down", [NE, D, D])
    b_down_d = din("b_down", [NE, D])
    final_g_d = din("final_norm_g", [1, D])
    ident_d = din("c_ident", [128, 128])
    invf_d = din("c_invf", [1, 8])
    poolm_d = din("c_poolm", [3, 128, 4, 128])
    tri_d = din("c_tri", [128, 128])
    eoff_d = din("c_eoff", [1, NE])
    trash_d = din("c_trash", [128, 1])
    lt_d = din("c_lt", [1, NE * NE])
    tab_d = din("c_tab", [1, 3 * NE])
    iotacp_d = din("c_iotacp", [128, 8])

    out_d = nc.dram_tensor("out", [S, D], F32, kind="ExternalOutput").ap()
    dbg = {}
    if stage < 99:
        dbg["u"] = nc.dram_tensor("dbg_u", [S, 2048], F32, kind="ExternalOutput").ap()
        dbg["qk"] = nc.dram_tensor("dbg_qk", [128, 8 * S], F32, kind="ExternalOutput").ap()
        dbg["x"] = nc.dram_tensor("dbg_x", [S, D], F32, kind="ExternalOutput").ap()

    xd_d = nc.dram_tensor("xd_scr", [NROWS + 128, D], BF16, kind="Internal").ap()
    top = ExitStack()
    with top:
        def sb(name, shape, dt, es=top):
            return es.enter_context(nc.sbuf_tensor(name, list(shape), dt))

        def ps(name, shape, dt, es=top):
            return es.enter_context(nc.psum_tensor(name, list(shape), dt))

        x_res = sb("x_res", [128, NT, D], F32)
        ident_f = sb("ident_f", [128, 128], F32)
        ident_b = sb("ident_b", [128, 128], BF16)
        ones_b = sb("ones_b", [128, 128], BF16)
        eps_t = sb("eps_t", [128, 1], F32)

        p1 = ExitStack()
        with p1:
            upool = sb("upool", [128, NT, 512], BF16, p1)
            qkT = sb("qkT", [128, 8, S], BF16, p1)
            v_all = sb("v_all", [128, NT, 4, 132], BF16, p1)
            mixT_w = sb("mixT_w", [128, 8, 2048], BF16, p1)
            w_in_sb = mixT_w

            pa = ExitStack()
            with pa:
                P = Prog(nc, pa)
                g_bc = sb("g_bc", [128, D], F32, pa)
                pos_i = sb("pos_i", [128, NT], I32, pa)
                pos_f = sb("pos_f", [128, NT], F32, pa)
                invf = sb("invf", [128, 8], F32, pa)
                ang = sb("ang", [128, NT, 8], F32, pa)
                argt = sb("argt", [128, NT, 8], F32, pa)
                argi = sb("argi", [128, NT, 8], I32, pa)
                cos_t = sb("cos_t", [128, NT, 8], F32, pa)
                sin_t = sb("sin_t", [128, NT, 8], F32, pa)
                ss = sb("ss", [128, NT], F32, pa)
                rstd = sb("rstd", [128, NT], F32, pa)
                junk = sb("junk", [128, D], BF16, pa)
                h_tok = [sb(f"h_tok{i}", [128, D], BF16, pa) for i in range(2)]
                hT = [sb(f"hT{i}", [128, 8, 128], BF16, pa) for i in range(2)]
                qk_rot = [sb(f"qk_rot{i}", [128, 1024], BF16, pa) for i in range(2)]
                rt = [sb(f"rt{i}", [128, 16, 8], F32, pa) for i in range(4)]
                udbg = sb("udbg", [128, 2048], F32, pa) if stage == 1 else None
                tp_ps = [ps(f"tp_ps{i}", [128, 8, 128], BF16, pa) for i in range(2)]
                u_ps = ps("u_ps", [128, 2048], F32, pa)
                qt_ps = ps("qt_ps", [128, 8, 128], BF16, pa)

                d_c = P.dsem()
                d_w = P.dsem()
                d_x = [P.dsem() for _ in range(4)]
                d_o = P.dsem()

                P.dma("sync", d_c, ident_f[:], ident_d)
                P.dma("sync", d_c, g_bc[:], attn_g_d.partition_broadcast(128))
                P.dma("sync", d_c, pos_i[:], pos_d)
                t_c = P.dma("sync", d_c, invf[:], invf_d.partition_broadcast(128))
                w_in_v = w_in_d.rearrange("(c p) f -> p c f", p=128)
                d_wn = [P.dsem() for _ in range(4)]
                t_wn = [P.dma("gpsimd", d_wn[nb], w_in_sb[:, :, nb * 512:(nb + 1) * 512], w_in_v[:, :, nb * 512:(nb + 1) * 512])
                        for nb in range(4)]
                t_idb = P.op("vector", "tensor_copy", ident_b[:], ident_f[:], waits=[t_c])
                P.op("vector", "memset", ones_b[:], 1.0)
                t_v1 = P.op("vector", "memset", v_all[:, :, :, 128:132], 1.0)
                t_eps = P.op("vector", "memset", eps_t[:], EPS)
                t = P.op("vector", "tensor_copy", pos_f[:], pos_i[:], waits=[t_c])
                t = P.op("vector", "tensor_tensor", ang[:], pos_f[:].unsqueeze(2).to_broadcast([128, NT, 8]),
                         invf[:].unsqueeze(1).to_broadcast([128, NT, 8]), ALU.mult, waits=[t])
                t_cs = _sincos(P, ang, sin_t, cos_t, argt, argi, [t])

                t_x = [None] * NT
                t_hT_free = [None, None]
                t_htok_free = [None, None]
                t_tp_free = [None, None]
                t_qkrot_free = [None, None]
                t_bank_free = [[], [], [], []]
                stA = {}
                stB = {}
                sd = {"qtps_free": None, "dbg_free": None}
                qk_v = u_ps[:, 512:1536].rearrange("p (a d) -> p a d", d=64)
                x1 = qk_v[:, :, 0:8]
                x2 = qk_v[:, :, 8:16]

                def stageA(i):
                    b = i % 2
                    t_x[i] = P.dma("sync", d_x[i % 4], x_res[:, i, :], x_d[i * 128:(i + 1) * 128, :],
                                   waits=[t_x[i - 4]] if i >= 4 else [])
                    t_ss = P.op("scalar", "activation", out=junk[:], in_=x_res[:, i, :], func=AF.Square,
                                accum_out=ss[:, i:i + 1], waits=[t_x[i], sd.get("junk")])
                    sd["junk"] = t_ss
                    t_r = _rstd(P, ss[:, i:i + 1], rstd[:, i:i + 1], D, [t_ss, t_eps], eps_t[:])
                    t_h = P.op("vector", "scalar_tensor_tensor", out=h_tok[b][:], in0=x_res[:, i, :], scalar=rstd[:, i:i + 1],
                               in1=g_bc[:], op0=ALU.mult, op1=ALU.mult, waits=[t_r, t_c, t_htok_free[b]])
                    t_tp = None
                    for c in range(8):
                        t_tp = P.op("tensor", "transpose", out=tp_ps[b][:, c, :], in_=h_tok[b][:, c * 128:(c + 1) * 128],
                                    identity=ident_b[:], waits=[t_h, t_idb, t_tp_free[b]], sig=(c == 7))
                    t_htok_free[b] = t_tp
                    t_hT = P.op("scalar", "activation", out=hT[b][:], in_=tp_ps[b][:], func=AF.Copy, waits=[t_tp, t_hT_free[b]])
                    t_tp_free[b] = t_hT
                    stA[i] = t_hT

                def stageB(i):
                    b = i % 2
                    t_hT = stA[i]
                    qr3 = qk_rot[b][:].rearrange("p (a d) -> p a d", d=64)
                    t_nb = []
                    for nb in range(4):
                        t_u = None
                        for c in range(8):
                            t_u = P.op("tensor", "matmul", u_ps[:, nb * 512:(nb + 1) * 512], lhsT=hT[b][:, c, :],
                                       rhs=w_in_sb[:, c, nb * 512:(nb + 1) * 512], start=(c == 0), stop=(c == 7),
                                       waits=[t_hT, t_wn[nb]] + t_bank_free[nb], sig=(c == 7))
                        t_nb.append(t_u)
                    t_hT_free[b] = t_nb[3]
                    t_up = P.op("scalar", "activation", out=upool[:, i, :], in_=u_ps[:, 0:512], func=AF.Copy, waits=[t_nb[0]])
                    t_bank_free[0] = [t_up]
                    t_uv = P.op("scalar", "activation", out=v_all[:, i, :, 0:128],
                                in_=u_ps[:, 1536:2048].rearrange("p (h v) -> p h v", h=4), func=AF.Copy, waits=[t_nb[3], t_v1])
                    t_bank_free[3] = [t_uv]
                    t_nr = P.op("scalar", "activation", out=qr3[:, :, 16:64], in_=qk_v[:, :, 16:64], func=AF.Copy,
                                waits=[t_nb[1], t_nb[2], t_qkrot_free[b]])
                    cb = cos_t[:, i:i + 1, :].to_broadcast([128, 16, 8])
                    sn = sin_t[:, i:i + 1, :].to_broadcast([128, 16, 8])
                    ta = P.op("vector", "tensor_tensor", rt[0][:], x1, cb, ALU.mult, waits=[t_nb[1], t_nb[2], t_cs] + sd.get("rt_free", []))
                    tb_ = P.op("vector", "tensor_tensor", rt[1][:], x2, sn, ALU.mult)
                    tc_ = P.op("vector", "tensor_tensor", rt[2][:], x2, cb, ALU.mult)
                    td_ = P.op("vector", "tensor_tensor", rt[3][:], x1, sn, ALU.mult)
                    te_ = P.op("vector", "tensor_tensor", qr3[:, :, 0:8], rt[0][:], rt[1][:], ALU.subtract,
                               waits=[ta, tb_, t_qkrot_free[b]])
                    tf_ = P.op("vector", "tensor_tensor", qr3[:, :, 8:16], rt[2][:], rt[3][:], ALU.add, waits=[tc_, td_])
                    t_bank_free[1] = [t_nr, td_]
                    t_bank_free[2] = [t_nr, td_]
                    sd["rt_free"] = [te_, tf_]
                    if stage == 1:
                        tdb = P.op("vector", "tensor_copy", udbg[:], u_ps[:], waits=t_nb + [sd["dbg_free"]])
                        for nb in range(4):
                            t_bank_free[nb] = t_bank_free[nb] + [tdb]
                        sd["dbg_free"] = P.dma("sync", d_o, dbg["u"][i * 128:(i + 1) * 128, :], udbg[:], waits=[tdb])
                    stB[i] = (t_nr, te_, tf_)

                def stageC(i):
                    b = i % 2
                    t_nr, te_, tf_ = stB[i]
                    t_qt = None
                    for j in range(8):
                        t_qt = P.op("tensor", "transpose", out=qt_ps[:, j, :], in_=qk_rot[b][:, j * 128:(j + 1) * 128],
                                    identity=ident_b[:], waits=[t_nr, te_, tf_, sd["qtps_free"]], sig=(j == 7))
                    t_qkrot_free[b] = t_qt
                    t_qT = P.op("scalar", "activation", out=qkT[:, :, i * 128:(i + 1) * 128], in_=qt_ps[:], func=AF.Copy, waits=[t_qt])
                    sd["qtps_free"] = t_qT

                stageA(0)
                for i in range(NT):
                    if i + 1 < NT:
                        stageA(i + 1)
                    stageB(i)
                    if i >= 1:
                        stageC(i - 1)
                stageC(NT - 1)
                t_dbg_free = sd["dbg_free"]
                if stage == 1:
                    P.wait("sync", [t_dbg_free])
                P.run()
            if stage == 1:
                pd = ExitStack()
                with pd:
                    P = Prog(nc, pd)
                    d_o = P.dsem()
                    tmp = sb("tmpdump", [128, 4, S], F32, pd)
                    qk_dst = dbg["qk"].rearrange("p (a s) -> p a s", a=8)
                    t2 = None
                    for hf in range(2):
                        t = P.op("vector", "tensor_copy", tmp[:], qkT[:, hf * 4:(hf + 1) * 4, :], waits=[t2])
                        for a in range(4):
                            t2 = P.dma("sync", d_o, qk_dst[:, hf * 4 + a, :], tmp[:, a, :], waits=[t])
                    P.wait("sync", [t2])
                    P.run()
                return nc
            w_out_sb = sb("w_out_sb", [128, 8, D], BF16, p1)
            mixT = mixT_w
            pb_ = ExitStack()
            with pb_:
                P = Prog(nc, pb_)
                poolm_sb = sb("poolm_sb", [128, 3, 4, 128], BF16, pb_)
                w_pool_sb = sb("w_pool_sb", [128, 4, 128], BF16, pb_)
                pscale = sb("pscale", [128, 4], F32, pb_)
                mixedT = [sb(f"mixedT{i}", [128, 512], BF16, pb_) for i in range(2)]
                mx_ps = [ps(f"mx_ps{i}", [128, 512], F32, pb_) for i in range(2)]
                yp_ps = [ps(f"yp_ps{i}", [128, 512], F32, pb_) for i in range(2)]
                d_c = P.dsem()
                d_w = P.dsem()
                d_wo = P.dsem()
                P.dma("gpsimd", d_w, poolm_sb[:], poolm_d.rearrange("k p g t -> p k g t"))
                t_w = P.dma("gpsimd", d_w, w_pool_sb[:], w_pool_d.rearrange("g c d -> c g d"))
                t_c = P.dma("sync", d_c, pscale[:], pool_scale_d)
                t_wo = P.dma("gpsimd", d_wo, w_out_sb[:], w_out_d.rearrange("(c p) f -> p c f", p=128))
                t_mixed_free = [None, None]
                t_mx_free = [None, None]
                t_yp_free = [None, None]
                n = 0
                for g in range(4):
                    for tb in range(4):
                        b = n % 2
                        n += 1
                        t_mx = None
                        for jj in range(4):
                            j = tb * 4 + jj
                            kind = 0 if j == 0 else 1
                            t_mx = P.op("tensor", "matmul", mx_ps[b][:, jj * 128:(jj + 1) * 128],
                                        lhsT=upool[:, j, g * 128:(g + 1) * 128], rhs=poolm_sb[:, kind, g, :],
                                        start=True, stop=(j == 0), waits=[t_w, t_mx_free[b]], sig=(j == 0 and jj == 3))
                            if j > 0:
                                t_mx = P.op("tensor", "matmul", mx_ps[b][:, jj * 128:(jj + 1) * 128],
                                            lhsT=upool[64:128, j - 1, g * 128:(g + 1) * 128], rhs=poolm_sb[64:128, 2, g, :],
                                            start=False, stop=True, sig=(jj == 3))
                        t_ev = P.op("scalar", "activation", out=mixedT[b][:], in_=mx_ps[b][:], func=AF.Copy,
                                    waits=[t_mx, t_mixed_free[b]])
                        t_mx_free[b] = t_ev
                        t_yp = P.op("tensor", "matmul", yp_ps[b][:], lhsT=w_pool_sb[:, g, :], rhs=mixedT[b][:],
                                    start=True, stop=True, waits=[t_ev, t_yp_free[b]])
                        t_mixed_free[b] = t_yp
                        t_yv = P.op("vector", "tensor_scalar", mixT[:, g, tb * 512:(tb + 1) * 512], yp_ps[b][:],
                                    pscale[:, g:g + 1], None, ALU.mult, waits=[t_yp, t_c])
                        t_yp_free[b] = t_yv
                P.wait("gpsimd", [t_wo])
                P.run()

            pc = ExitStack()
            with pc:
                P = Prog(nc, pc)
                lam_sb = sb("lam_sb", [128, 256], F32, pc)
                lprod = sb("lprod", [128, 2, 64], F32, pc)
                lsum = sb("lsum", [128, 2], F32, pc)
                neglam = sb("neglam", [128, 1], F32, pc)
                gsub = sb("gsub", [128, 128], F32, pc)
                pT = [sb(f"pT{i}", [128, 512], BF16, pc) for i in range(3)]
                o1 = [sb(f"o1_{i}", [128, 4, 128], F32, pc) for i in range(2)]
                osb = [sb(f"osb{i}", [128, 128], F32, pc) for i in range(2)]
                ojunk = sb("ojunk", [128, 128], BF16, pc)
                y_tok = [sb(f"y_tok{i}", [128, 128], BF16, pc) for i in range(8)]
                rl = sb("rl", [128, 64], F32, pc)
                rl2 = sb("rl2", [128, 64], F32, pc)
                oss = sb("oss", [128, 64], F32, pc)
                orstd = sb("orstd", [128, 64], F32, pc)
                s_ps = [ps(f"s_ps{i}", [128, 512], F32, pc) for i in range(2)]
                acc_ps = [[ps(f"acc_ps{i}_{k}", [128, 512], F32, pc) for k in range(2)] for i in range(2)]
                yt_ps = ps("yt_ps", [128, 4, 128], BF16, pc)
                d_c = P.dsem()
                P.dma("sync", d_c, lam_sb[:], lam_d.partition_broadcast(128))
                t_c = P.dma("sync", d_c, gsub[:], subln_g_d.partition_broadcast(128))
                zt = sb("zt", [128, 4096], BF16, pc)
                d_z = P.dsem()
                t_zm = P.op("gpsimd", "memset", zt[:], 0.0)
                t_zd = None
                for r0 in range(0, NROWS + 128, 512):
                    t_zd = P.dma("sync", d_z, xd_d[r0:r0 + 512, :].rearrange("(p s) d -> p (s d)", s=4), zt[:], waits=[t_zm])
                lv = lam_sb[:].rearrange("p (a b d) -> p a b d", a=2, b=2)
                t = P.op("vector", "tensor_tensor", lprod[:], lv[:, :, 0, :], lv[:, :, 1, :], ALU.mult, waits=[t_c])
                t = P.op("vector", "tensor_reduce", lsum[:], lprod[:], AX.X, ALU.add, waits=[t])
                t = P.op("scalar", "activation", out=lsum[:], in_=lsum[:], func=AF.Exp, waits=[t])
                t = P.op("vector", "tensor_tensor", neglam[:], lsum[:, 1:2], lsum[:, 0:1], ALU.subtract, waits=[t])
                t_lam = P.op("vector", "tensor_scalar_add", neglam[:], neglam[:], -LAM_INIT, waits=[t])
                t_gs = P.op("vector", "tensor_scalar_mul", gsub[:], gsub[:], 1.0 - LAM_INIT, waits=[t_c])

                s_ring = Ring(s_ps)
                pT_ring = Ring(pT)
                t_acc_free = [[None, None], [None, None]]
                t_o1_free = [None, None]
                t_osb_free = [None, None]
                t_ytok_free = [None] * 8
                st8 = {"ytps_free": None, "nq": 0}

                rounds = []
                for h in range(4):
                    for qb in range(4):
                        for m in range(2):
                            rounds.append(dict(h=h, qb=qb, m=m, ab=len(rounds) % 2, ob=(h * 4 + qb) % 2,
                                               bank_started=[False, False], t_last=[None] * 4))
                steps = [(r, kt) for r in rounds for kt in range(4 * r["qb"] + 4)]

                def emit_S(r, kt):
                    h, qb, m = r["h"], r["qb"], r["m"]
                    c0 = max(0, kt - 4 * qb)
                    si, sbuf, sfr = s_ring.get()
                    pi, pbuf, pfr = pT_ring.get()
                    t_s = P.op("tensor", "matmul", sbuf[:, c0 * 128:512],
                               lhsT=qkT[m * 64:(m + 1) * 64, 4 + h, kt * 128:(kt + 1) * 128],
                               rhs=qkT[m * 64:(m + 1) * 64, h, qb * 512 + c0 * 128:(qb + 1) * 512],
                               start=True, stop=True, waits=[sfr])
                    t_e = P.op("scalar", "activation", out=pbuf[:, c0 * 128:512], in_=sbuf[:, c0 * 128:512],
                               func=AF.Exp, scale=0.125, waits=[t_s, pfr])
                    s_ring.release(si, t_e)
                    if kt >= 4 * qb:
                        t_e = P.op("vector", "memset", pbuf[64:128, c0 * 128:c0 * 128 + 64], 0.0, waits=[t_e])
                    return (c0, pi, pbuf, t_e)

                def emit_PV(r, kt, sres):
                    h, qb, ab = r["h"], r["qb"], r["ab"]
                    c0, pi, pbuf, t_e = sres
                    t_pv = None
                    for ql in range(c0, 4):
                        bk = ql // 2
                        off = (ql % 2) * 256
                        last = (kt == 4 * qb + ql)
                        t_pv = P.op("tensor", "matmul", acc_ps[ab][bk][:, off:off + 129],
                                    lhsT=pbuf[:, ql * 128:(ql + 1) * 128], rhs=v_all[:, kt, h, 0:129],
                                    start=(not r["bank_started"][bk]), stop=last, skip_group_check=True,
                                    waits=[t_e, t_acc_free[ab][bk]], sig=(last or ql == 3))
                        r["bank_started"][bk] = True
                        if last:
                            r["t_last"][ql] = t_pv
                    pT_ring.release(pi, t_pv)

                def emit_evac(r):
                    h, qb, m, ab, ob = r["h"], r["qb"], r["m"], r["ab"], r["ob"]
                    t_last = r["t_last"]
                    t_ev_bank = [None, None]
                    for ql in range(4):
                        bk = ql // 2
                        off = (ql % 2) * 256
                        acc = acc_ps[ab][bk]
                        if m == 0:
                            ci = (st8["nq"] + ql) % 64
                            t_r = P.op("vector", "reciprocal", rl[:, ci:ci + 1], acc[:, off + 128:off + 129], waits=[t_last[ql], t_last[2 * bk + 1]])
                            t_o = P.op("vector", "tensor_scalar", o1[ob][:, ql, :], acc[:, off:off + 128], rl[:, ci:ci + 1], None,
                                       ALU.mult, waits=[t_r, t_o1_free[ob]])
                            t_ev_bank[bk] = t_o
                        else:
                            nq = st8["nq"]
                            ci = nq % 64
                            yb = nq % 8
                            ob2 = nq % 2
                            st8["nq"] = nq + 1
                            t_r = P.op("vector", "reciprocal", rl2[:, ci:ci + 1], acc[:, off + 128:off + 129], waits=[t_last[ql], t_last[2 * bk + 1]])
                            t_r = P.op("vector", "tensor_tensor", rl2[:, ci:ci + 1], rl2[:, ci:ci + 1], neglam[:], ALU.mult,
                                       waits=[t_r, t_lam])
                            t_o = P.op("vector", "scalar_tensor_tensor", out=osb[ob2][:], in0=acc[:, off:off + 128],
                                       scalar=rl2[:, ci:ci + 1], in1=o1[ob][:, ql, :], op0=ALU.mult, op1=ALU.add,
                                       waits=[t_r, t_osb_free[ob2]])
                            t_ev_bank[bk] = t_o
                            t_ss = P.op("scalar", "activation", out=ojunk[:], in_=osb[ob2][:], func=AF.Square,
                                        accum_out=oss[:, ci:ci + 1], waits=[t_o])
                            t_rs = _rstd(P, oss[:, ci:ci + 1], orstd[:, ci:ci + 1], 128, [t_ss], eps_t[:])
                            t_y = P.op("vector", "scalar_tensor_tensor", out=y_tok[yb][:], in0=osb[ob2][:],
                                       scalar=orstd[:, ci:ci + 1], in1=gsub[:], op0=ALU.mult, op1=ALU.mult,
                                       waits=[t_rs, t_gs, t_ytok_free[yb]])
                            t_osb_free[ob2] = t_y
                            r.setdefault("ty", []).append((ql, yb, t_y))
                            if ql == 3:
                                t_o1_free[ob] = t_o
                    t_acc_free[ab] = list(t_ev_bank)

                def emit_ytrans(r):
                    if r["m"] == 0:
                        return
                    h, qb = r["h"], r["qb"]
                    t_t = None
                    for (ql, yb, t_y) in r["ty"]:
                        t_t = P.op("tensor", "transpose", out=yt_ps[:, ql, :], in_=y_tok[yb][:], identity=ident_b[:],
                                   waits=[t_y, st8["ytps_free"] if ql == 0 else None])
                        t_ytok_free[yb] = t_t
                    t_cp = P.op("scalar", "activation", out=mixT[:, 4 + h, qb * 512:(qb + 1) * 512],
                                in_=yt_ps[:].rearrange("p a b -> p (a b)"), func=AF.Copy, waits=[t_t])
                    st8["ytps_free"] = t_cp

                prev = None
                pend_tr = None
                for j, (r, kt) in enumerate(steps):
                    sres = emit_S(r, kt)
                    if prev is not None:
                        pr_, pkt, psres = prev
                        emit_PV(pr_, pkt, psres)
                        if pkt == 4 * pr_["qb"] + 3:
                            if pend_tr is not None:
                                emit_ytrans(pend_tr)
                            emit_evac(pr_)
                            pend_tr = pr_
                    prev = (r, kt, sres)
                pr_, pkt, psres = prev
                emit_PV(pr_, pkt, psres)
                if pend_tr is not None:
                    emit_ytrans(pend_tr)
                emit_evac(pr_)
                emit_ytrans(pr_)
                P.wait("sync", [t_zd])
                P.run()

            pw = ExitStack()
            with pw:
                P = Prog(nc, pw)
                wo_ps = [ps(f"wo_ps{i}", [128, 512], F32, pw) for i in range(4)]
                d_o = P.dsem()
                t_free = [None] * 4
                n = 0
                t_dump = None
                for i in range(NT):
                    tv = None
                    for hf in range(2):
                        b = n % 4
                        n += 1
                        t_m = None
                        for c in range(8):
                            t_m = P.op("tensor", "matmul", wo_ps[b][:], lhsT=mixT[:, c, i * 128:(i + 1) * 128],
                                       rhs=w_out_sb[:, c, hf * 512:(hf + 1) * 512], start=(c == 0), stop=(c == 7),
                                       waits=[t_free[b]], sig=(c == 7))
                        tv = P.op("vector", "tensor_tensor", x_res[:, i, hf * 512:(hf + 1) * 512], wo_ps[b][:],
                                  x_res[:, i, hf * 512:(hf + 1) * 512], ALU.add, waits=[t_m])
                        t_free[b] = tv
                    if stage == 2:
                        t_dump = P.dma("sync", d_o, dbg["x"][i * 128:(i + 1) * 128, :], x_res[:, i, :], waits=[tv])
                if stage == 2:
                    P.wait("sync", [t_dump])
                P.run()
            if stage == 2:
                return nc
        p2 = ExitStack()
        with p2:
            kcT = sb("kcT", [128, 8, MEM], BF16, p2)
            vc = sb("vc", [128, 2, D], BF16, p2)
            w_cq_sb = sb("w_cq_sb", [128, 8, D], BF16, p2)
            w_co_sb = sb("w_co_sb", [128, 8, D], BF16, p2)
            g2_bc = sb("g2_bc", [128, D], F32, p2)
            pm_ = ExitStack()
            with pm_:
                P = Prog(nc, pm_)
                w_ckv_sb = sb("w_ckv_sb", [128, 8, 2 * D], BF16, pm_)
                mem_sb = sb("mem_sb", [128, 2, D], F32, pm_)
                gm_bc = sb("gm_bc", [128, D], F32, pm_)
                memT = sb("memT", [128, 8, MEM], BF16, pm_)
                mss = sb("mss", [128, 2], F32, pm_)
                mrstd = sb("mrstd", [128, 2], F32, pm_)
                junk = sb("junk2", [128, D], BF16, pm_)
                h_tok = [sb(f"m_tok{i}", [128, D], BF16, pm_) for i in range(2)]
                tp_ps = [ps(f"tp2_ps{i}", [128, 8, 128], BF16, pm_) for i in range(2)]
                gen = Ring([ps(f"gen2a_{i}", [128, 512], F32, pm_) for i in range(4)])
                d_c = P.dsem()
                d_w = P.dsem()
                d_w2 = P.dsem()
                P.dma("sync", d_c, mem_sb[:], mem_d.rearrange("(t p) d -> p t d", p=128))
                P.dma("sync", d_c, g2_bc[:], xattn_g_d.partition_broadcast(128))
                t_c = P.dma("sync", d_c, gm_bc[:], mem_g_d.partition_broadcast(128))
                w_ckv_v = w_ckv_d.rearrange("(c p) f -> p c f", p=128)
                t_wk = P.dma("gpsimd", d_w, w_ckv_sb[:, :, 0:D], w_ckv_v[:, :, 0:D])
                d_wv = P.dsem()
                t_wv = P.dma("gpsimd", d_wv, w_ckv_sb[:, :, D:2 * D], w_ckv_v[:, :, D:2 * D])
                P.dma("gpsimd", d_w2, w_cq_sb[:], w_cq_d.rearrange("(c p) f -> p c f", p=128))
                t_w2 = P.dma("gpsimd", d_w2, w_co_sb[:], w_co_d.rearrange("(c p) f -> p c f", p=128))
                t_mT = []
                for mt in range(2):
                    t_ss = P.op("scalar", "activation", out=junk[:], in_=mem_sb[:, mt, :], func=AF.Square,
                                accum_out=mss[:, mt:mt + 1], waits=[t_c])
                    t_r = _rstd(P, mss[:, mt:mt + 1], mrstd[:, mt:mt + 1], D, [t_ss], eps_t[:])
                    t_h = P.op("vector", "scalar_tensor_tensor", out=h_tok[mt][:], in0=mem_sb[:, mt, :], scalar=mrstd[:, mt:mt + 1],
                               in1=gm_bc[:], op0=ALU.mult, op1=ALU.mult, waits=[t_r, t_c])
                    t_tp = None
                    for c in range(8):
                        t_tp = P.op("tensor", "transpose", out=tp_ps[mt][:, c, :], in_=h_tok[mt][:, c * 128:(c + 1) * 128],
                                    identity=ident_b[:], waits=[t_h], sig=(c == 7))
                    t_mT.append(P.op("scalar", "activation", out=memT[:, :, mt * 128:(mt + 1) * 128], in_=tp_ps[mt][:],
                                     func=AF.Copy, waits=[t_tp]))
                for fch in range(8):
                    bi, buf, fr = gen.get()
                    t_m = None
                    for c in range(8):
                        t_m = P.op("tensor", "matmul", buf[:, 0:MEM], lhsT=w_ckv_sb[:, c, fch * 128:(fch + 1) * 128],
                                   rhs=memT[:, c, :], start=(c == 0), stop=(c == 7), waits=[t_wk, fr] + t_mT, sig=(c == 7))
                    gen.release(bi, P.op("scalar", "activation", out=kcT[:, fch, :], in_=buf[:, 0:MEM], func=AF.Copy, waits=[t_m]))
                for mt in range(2):
                    for hf in range(2):
                        bi, buf, fr = gen.get()
                        t_m = None
                        for c in range(8):
                            t_m = P.op("tensor", "matmul", buf[:], lhsT=memT[:, c, mt * 128:(mt + 1) * 128],
                                       rhs=w_ckv_sb[:, c, D + hf * 512:D + (hf + 1) * 512], start=(c == 0), stop=(c == 7),
                                       waits=[t_wv, fr], sig=(c == 7))
                        gen.release(bi, P.op("vector", "tensor_copy", vc[:, mt, hf * 512:(hf + 1) * 512], buf[:], waits=[t_m]))
                P.wait("gpsimd", [t_w2])
                P.run()

            px = ExitStack()
            with px:
                P = Prog(nc, px)
                h2T = sb("h2T", [128, 8, S], BF16, px)
                qcT = sb("qcT", [128, 8, S], BF16, px)
                xss = sb("xss", [128, NT], F32, px)
                xrstd = sb("xrstd", [128, NT], F32, px)
                junk = sb("junk3", [128, D], BF16, px)
                h_tok = [sb(f"x_tok{i}", [128, D], BF16, px) for i in range(2)]
                pT = [[sb(f"cpT{i}_{k}", [128, 512], BF16, px) for k in range(2)] for i in range(2)]
                rl = [sb(f"crl{i}", [128, 512], F32, px) for i in range(2)]
                tp_ps = [ps(f"tp3_ps{i}", [128, 8, 128], BF16, px) for i in range(2)]
                gen = Ring([ps(f"gen2b_{i}", [128, 512], F32, px) for i in range(6)])
                d_o = P.dsem()
                t_htok_free = [None, None]
                t_tp_free = [None, None]
                t_pT_free = [None, None]
                t_rl_free = [None, None]
                sd2 = {"nh": 0, "dump": None}
                hT_tok = {}
                q_tok = {}

                def stA(tb):
                    t_hT = []
                    for ii in range(4):
                        i = tb * 4 + ii
                        b = i % 2
                        t_ss = P.op("scalar", "activation", out=junk[:], in_=x_res[:, i, :], func=AF.Square,
                                    accum_out=xss[:, i:i + 1])
                        t_r = _rstd(P, xss[:, i:i + 1], xrstd[:, i:i + 1], D, [t_ss], eps_t[:])
                        t_h = P.op("vector", "scalar_tensor_tensor", out=h_tok[b][:], in0=x_res[:, i, :], scalar=xrstd[:, i:i + 1],
                                   in1=g2_bc[:], op0=ALU.mult, op1=ALU.mult, waits=[t_r, t_htok_free[b]])
                        t_tp = None
                        for c in range(8):
                            t_tp = P.op("tensor", "transpose", out=tp_ps[b][:, c, :], in_=h_tok[b][:, c * 128:(c + 1) * 128],
                                        identity=ident_b[:], waits=[t_h, t_tp_free[b]], sig=(c == 7))
                        t_htok_free[b] = t_tp
                        t_cp = P.op("scalar", "activation", out=h2T[:, :, i * 128:(i + 1) * 128], in_=tp_ps[b][:], func=AF.Copy,
                                    waits=[t_tp])
                        t_tp_free[b] = t_cp
                        t_hT.append(t_cp)
                    hT_tok[tb] = t_hT

                def stQ(tb):
                    tsl = slice(tb * 512, (tb + 1) * 512)
                    t_q = []
                    for fch in range(8):
                        bi, buf, fr = gen.get()
                        t_m = None
                        for c in range(8):
                            t_m = P.op("tensor", "matmul", buf[:], lhsT=w_cq_sb[:, c, fch * 128:(fch + 1) * 128],
                                       rhs=h2T[:, c, tsl], start=(c == 0), stop=(c == 7), waits=hT_tok[tb] + [fr], sig=(c == 7))
                        if fch % 2 == 0:
                            t_e = P.op("scalar", "activation", out=qcT[:, fch, tsl], in_=buf[:], func=AF.Copy, waits=[t_m])
                        else:
                            t_e = P.op("vector", "tensor_copy", qcT[:, fch, tsl], buf[:], waits=[t_m])
                        gen.release(bi, t_e)
                        t_q.append(t_e)
                    q_tok[tb] = t_q

                def stS(tb, hh):
                    tsl = slice(tb * 512, (tb + 1) * 512)
                    t_q = q_tok[tb]
                    pb = sd2["nh"] % 2
                    sd2["nh"] += 1
                    t_p = []
                    for mt in range(2):
                        bi, buf, fr = gen.get()
                        t_m = None
                        for j in range(2):
                            t_m = P.op("tensor", "matmul", buf[:], lhsT=kcT[:, 2 * hh + j, mt * 128:(mt + 1) * 128],
                                       rhs=qcT[:, 2 * hh + j, tsl], start=(j == 0), stop=(j == 1),
                                       waits=[t_q[2 * hh], t_q[2 * hh + 1], fr], sig=(j == 1))
                        t_e = P.op("scalar", "activation", out=pT[pb][mt][:], in_=buf[:], func=AF.Exp, scale=1.0 / 16.0,
                                   waits=[t_m, t_pT_free[pb]])
                        gen.release(bi, t_e)
                        t_p.append(t_e)
                    return (pb, t_p)

                def stL(tb, hh, sres):
                    tsl = slice(tb * 512, (tb + 1) * 512)
                    t_q = q_tok[tb]
                    pb, t_p = sres
                    bl, lbuf, fr = gen.get()
                    t_l = None
                    for mt in range(2):
                        t_l = P.op("tensor", "matmul", lbuf[:], lhsT=ones_b[:], rhs=pT[pb][mt][:], start=(mt == 0), stop=(mt == 1),
                                   waits=t_p + [fr], sig=(mt == 1))
                    t_rl = P.op("vector", "reciprocal", rl[pb][:], lbuf[:], waits=[t_l, t_rl_free[pb]])
                    gen.release(bl, t_rl)
                    t_o = None
                    t_m = None
                    for j in range(2):
                        bo, obuf, fr = gen.get()
                        for mt in range(2):
                            t_m = P.op("tensor", "matmul", obuf[:], lhsT=vc[:, mt, (2 * hh + j) * 128:(2 * hh + j + 1) * 128],
                                       rhs=pT[pb][mt][:], start=(mt == 0), stop=(mt == 1), waits=[fr], sig=(mt == 1))
                        t_o = P.op("vector", "tensor_tensor", qcT[:, 2 * hh + j, tsl], obuf[:], rl[pb][:], ALU.mult, waits=[t_m, t_rl])
                        gen.release(bo, t_o)
                        t_q[2 * hh + j] = t_o
                    t_pT_free[pb] = t_m
                    t_rl_free[pb] = t_o

                def stATT(tb):
                    prev = None
                    for hh in range(4):
                        sres = stS(tb, hh)
                        if prev is not None:
                            stL(tb, prev[0], prev[1])
                        prev = (hh, sres)
                    stL(tb, prev[0], prev[1])

                def stO(tb):
                    t_q = q_tok[tb]
                    for ii in range(4):
                        i = tb * 4 + ii
                        tv = None
                        for hf in range(2):
                            bi, buf, fr = gen.get()
                            t_m = None
                            for c in range(8):
                                t_m = P.op("tensor", "matmul", buf[:], lhsT=qcT[:, c, i * 128:(i + 1) * 128],
                                           rhs=w_co_sb[:, c, hf * 512:(hf + 1) * 512], start=(c == 0), stop=(c == 7),
                                           waits=t_q + [fr], sig=(c == 7))
                            tv = P.op("vector", "tensor_tensor", x_res[:, i, hf * 512:(hf + 1) * 512], buf[:],
                                      x_res[:, i, hf * 512:(hf + 1) * 512], ALU.add, waits=[t_m])
                            gen.release(bi, tv)
                        if stage == 3:
                            sd2["dump"] = P.dma("sync", d_o, dbg["x"][i * 128:(i + 1) * 128, :], x_res[:, i, :], waits=[tv])

                stA(0)
                stQ(0)
                stA(1)
                for tb in range(4):
                    stATT(tb)
                    if tb + 1 < 4:
                        stQ(tb + 1)
                    stO(tb)
                    if tb + 2 < 4:
                        stA(tb + 2)
                t_dump = sd2["dump"]
                if stage == 3:
                    P.wait("sync", [t_dump])
                P.run()
            if stage == 3:
                return nc
        yd_d = nc.dram_tensor("yd_scr", [NROWS + 128, D], F32, kind="Internal").ap()
        x2_d = nc.dram_tensor("x2_scr", [S, D], F32, kind="Internal").ap()
        p3 = ExitStack()
        with p3:
            idx = sb("idx", [128, 4, NT], I32, p3)
            gates = sb("gates", [128, 4, NT], F32, p3)
            bgu_sb = sb("bgu_sb", [128, NE * 16], F32, p3)
            wd = [sb(f"wd{i}", [128, 8, D], BF16, p3) for i in range(2)]
            xrb = x_res[:].bitcast(BF16)
            wgu = [xrb[:, sl * 8:(sl + 1) * 8, :] for sl in range(2)]
            d_wg = [Sem(p3.enter_context(nc.semaphore(f"xwg{i}"))) for i in range(2)]
            d_wd = [Sem(p3.enter_context(nc.semaphore(f"xwd{i}"))) for i in range(2)]
            wtok = {}
            idxw = sb("idxw", [128, NE, 8], I32, p3)
            idxb = sb("idxb", [128, NE], I32, p3)
            oh_r = sb("oh_r", [128, NE, NE], F32, p3)
            d_bdw = [Sem(p3.enter_context(nc.semaphore(f"xbd{i}"))) for i in range(2)]
            wgu_rows = w_gu_d.rearrange("e r f -> (e r) f")
            wdn_rows = w_down_d.rearrange("e r f -> (e r) f")

            def issue_wloads(P, e, waits_g=(), waits_d=()):
                sl = e % 2
                t_g = None
                for c in range(8):
                    t_g = P.dma("gpsimd", d_wg[sl], wgu[sl][:, c, :], wgu_rows, waits=list(waits_g) if c == 0 else [],
                                meth="indirect_dma_start", out_offset=None,
                                in_offset=bass.IndirectOffsetOnAxis(ap=idxw[:, e, c:c + 1], axis=0))
                t_d = None
                for c in range(8):
                    t_d = P.dma("gpsimd", d_wd[sl], wd[sl][:, c, :], wdn_rows, waits=list(waits_d) if c == 0 else [],
                                meth="indirect_dma_start", out_offset=None,
                                in_offset=bass.IndirectOffsetOnAxis(ap=idxw[:, e, c:c + 1], axis=0))
                wtok[e] = (t_g, t_d)
            pr = ExitStack()
            with pr:
                P = Prog(nc, pr)
                h3_all = sb("h3_all", [128, NT, D], BF16, pr)
                g3_bc = sb("g3_bc", [128, D], F32, pr)
                wr_sb = sb("wr_sb", [128, 8, NE], F32, pr)
                br_bc = sb("br_bc", [128, NE], F32, pr)
                ltm = sb("ltm", [128, NE, NE], F32, pr)
                tab = sb("tab", [128, 3 * NE], F32, pr)
                iotacp = sb("iotacp", [128, 8], F32, pr)
                cnt = sb("cnt", [128, NE], F32, pr)
                rank = sb("rank", [128, NE], F32, pr)
                base_e = sb("base_e", [128, NE], F32, pr)
                cap_e = sb("cap_e", [128, NE], F32, pr)
                perm = sb("perm", [128, NE], F32, pr)
                idxwf = sb("idxwf", [128, NE, 8], F32, pr)
                tri_f = sb("tri_f", [128, 128], F32, pr)
                trash = sb("trash", [128, 1], F32, pr)
                zrow = sb("zrow", [128, D], F32, pr)
                tri_b = sb("tri_b", [128, 128], BF16, pr)
                bgu_raw = sb("bgu_raw", [128, 4, 128], F32, pr)
                rss = sb("rss", [128, NT], F32, pr)
                rrstd = sb("rrstd", [128, NT], F32, pr)
                junk = sb("junk4", [128, D], BF16, pr)
                h3f = [sb(f"h3f{i}", [128, D], F32, pr) for i in range(2)]
                h3T = [sb(f"h3T{i}", [128, 8, 128], F32, pr) for i in range(2)]
                lg = sb("lg", [128, NT, NE], F32, pr)
                work = sb("work", [128, NT, NE], F32, pr)
                cm1 = h3f[0][:].rearrange("p (a b) -> p a b", b=NE)
                cm2 = h3f[1][:].rearrange("p (a b) -> p a b", b=NE)
                ovff = work
                oh = [sb(f"oh{k}", [128, NT, NE], F32, pr) for k in range(4)]
                mask_f = sb("mask_f", [128, NT, NE], F32, pr)
                mask_b = sb("mask_b", [128, NT * NE], BF16, pr)
                mv = sb("mv", [128, 4, NT], F32, pr)
                evk = sb("evk", [128, 4, NT], F32, pr)
                den = sb("den", [128, NT], F32, pr)
                tot = sb("tot", [128, NT, NE], F32, pr)
                basec = sb("basec", [128, NT, NE], F32, pr)
                pos = sb("rpos", [128, NT, NE], F32, pr)
                dest = sb("rdest", [128, NT, NE], F32, pr)
                tmp3 = sb("tmp3", [128, NT, NE], F32, pr)
                destk = sb("destk", [128, 4, NT], F32, pr)
                posk = sb("posk", [128, 4, NT], F32, pr)
                ovf = sb("ovf", [128, 4, NT], F32, pr)
                tpf = [[ps(f"tpf{i}_{k}", [128, 4, 128], F32, pr) for k in range(2)] for i in range(2)]
                lg_ps = ps("lg_ps", [128, NT, NE], F32, pr)
                tot_ps = ps("tot_ps", [128, NT * NE], F32, pr)
                win_ps = ps("win_ps", [128, NT * NE], F32, pr)
                d_c = P.dsem()
                d_x2 = P.dsem()
                d_sc = [P.dsem() for _ in range(4)]
                P.dma("sync", d_c, g3_bc[:], ffn_g_d.partition_broadcast(128))
                P.dma("sync", d_c, wr_sb[:], w_router_d.rearrange("(c p) e -> p c e", p=128))
                P.dma("sync", d_c, br_bc[:], b_router_d.partition_broadcast(128))
                P.dma("sync", d_c, ltm[:].rearrange("p a b -> p (a b)"), lt_d.partition_broadcast(128))
                P.dma("sync", d_c, tab[:], tab_d.partition_broadcast(128))
                P.dma("sync", d_c, iotacp[:], iotacp_d)
                P.dma("sync", d_c, tri_f[:], tri_d)
                P.dma("sync", d_c, trash[:], trash_d)
                t_c = P.dma("sync", d_c, bgu_raw[:], b_gu_d.rearrange("(a p) f -> p a f", p=128))
                t_tri = P.op("vector", "tensor_copy", tri_b[:], tri_f[:], waits=[t_c])
                t_z = P.op("vector", "memset", zrow[:], 0.0)
                t_x2 = P.dma("sync", d_x2, yd_d[NROWS:NROWS + 128, :], zrow[:], waits=[t_z])
                for a in range(4):
                    tt = P.op("tensor", "transpose", out=tpf[0][0][:, 0, :], in_=bgu_raw[:, a, :], identity=ident_f[:],
                              waits=[t_c] if a == 0 else [tcp])
                    tcp = P.op("vector", "tensor_copy", bgu_sb[:, a * 128:(a + 1) * 128], tpf[0][0][:, 0, :], waits=[tt])
                t_h3f_free = [None, None]
                t_tpf_free = [tcp, None]
                t_h3T_free = [None, None]
                t_x2 = None
                t_lg = None
                for i in range(NT):
                    b = i % 2
                    t_x2 = P.dma("sync", d_x2, x2_d[i * 128:(i + 1) * 128, :], x_res[:, i, :])
                    t_ss = P.op("scalar", "activation", out=junk[:], in_=x_res[:, i, :], func=AF.Square, accum_out=rss[:, i:i + 1])
                    t_r = _rstd(P, rss[:, i:i + 1], rrstd[:, i:i + 1], D, [t_ss], eps_t[:])
                    t_h = P.op("vector", "scalar_tensor_tensor", out=h3f[b][:], in0=x_res[:, i, :], scalar=rrstd[:, i:i + 1],
                               in1=g3_bc[:], op0=ALU.mult, op1=ALU.mult, waits=[t_r, t_c, t_h3f_free[b]])
                    t_hb = P.op("scalar", "activation", out=h3_all[:, i, :], in_=h3f[b][:], func=AF.Copy, waits=[t_h])
                    t_tp = None
                    for c in range(8):
                        t_tp = P.op("tensor", "transpose", out=tpf[b][c // 4][:, c % 4, :], in_=h3f[b][:, c * 128:(c + 1) * 128],
                                    identity=ident_f[:], waits=[t_h, t_tpf_free[b]], sig=(c == 7))
                    t_cp0 = P.op("vector", "tensor_copy", h3T[b][:, 0:4, :], tpf[b][0][:], waits=[t_tp, t_h3T_free[b]])
                    t_cp1 = P.op("vector", "tensor_copy", h3T[b][:, 4:8, :], tpf[b][1][:], waits=[t_tp])
                    t_tpf_free[b] = t_cp1
                    for c in range(8):
                        t_lg = P.op("tensor", "matmul", lg_ps[:, i, :], lhsT=h3T[b][:, c, :], rhs=wr_sb[:, c, :],
                                    start=(c == 0), stop=(c == 7), waits=[t_cp0, t_cp1], sig=(c == 7))
                    t_h3T_free[b] = t_lg
                    t_h3f_free[b] = t_lg
                    P.wait("vector", [t_hb])
                t_xres_free = [t_x2, t_ss, t_h]
                t = P.op("vector", "tensor_tensor", lg[:], lg_ps[:], br_bc[:].unsqueeze(1).to_broadcast([128, NT, NE]), ALU.add,
                         waits=[t_lg, t_c])
                t = P.op("vector", "tensor_copy", work[:], lg[:], waits=[t])
                for k in range(4):
                    t = P.op("vector", "tensor_reduce", mv[:, k, :], work[:], AX.X, ALU.max, waits=[t])
                    t = P.op("vector", "tensor_tensor", oh[k][:], work[:], mv[:, k, :].unsqueeze(2).to_broadcast([128, NT, NE]),
                             ALU.is_equal, waits=[t])
                    t = P.op("vector", "scalar_tensor_tensor", out=work[:], in0=oh[k][:], scalar=-1e30, in1=work[:],
                             op0=ALU.mult, op1=ALU.add, waits=[t])
                t = P.op("vector", "tensor_tensor", mask_f[:], oh[0][:], oh[1][:], ALU.add, waits=[t])
                t = P.op("vector", "tensor_tensor", mask_f[:], mask_f[:], oh[2][:], ALU.add, waits=[t])
                t = P.op("vector", "tensor_tensor", mask_f[:], mask_f[:], oh[3][:], ALU.add, waits=[t])
                t_mb = P.op("vector", "tensor_copy", mask_b[:], mask_f[:].rearrange("p a b -> p (a b)"), waits=[t])
                t = P.op("vector", "tensor_tensor", evk[:], mv[:], mv[:, 0:1, :].to_broadcast([128, 4, NT]), ALU.subtract, waits=[t_mb])
                t = P.op("scalar", "activation", out=evk[:], in_=evk[:], func=AF.Exp, waits=[t])
                t = P.op("vector", "tensor_reduce", den[:], evk[:].rearrange("p k t -> p t k"), AX.X, ALU.add, waits=[t])
                t = P.op("vector", "reciprocal", den[:], den[:], waits=[t])
                t_g = P.op("vector", "tensor_tensor", gates[:], evk[:], den[:].unsqueeze(1).to_broadcast([128, 4, NT]), ALU.mult, waits=[t])
                t_tot = P.op("tensor", "matmul", tot_ps[:], lhsT=ones_b[:], rhs=mask_b[:], start=True, stop=True, waits=[t_mb])
                t_win = P.op("tensor", "matmul", win_ps[:], lhsT=tri_b[:], rhs=mask_b[:], start=True, stop=True, waits=[t_tri])
                t = P.op("vector", "tensor_copy", tot[:], tot_ps[:].rearrange("p (a b) -> p a b", b=NE), waits=[t_tot])
                t = P.op("vector", "memset", basec[:, 0, :], 0.0, waits=[t])
                for i in range(1, NT):
                    t = P.op("vector", "tensor_tensor", basec[:, i, :], basec[:, i - 1, :], tot[:, i - 1, :], ALU.add, waits=[t])
                t = P.op("vector", "tensor_tensor", pos[:], win_ps[:].rearrange("p (a b) -> p a b", b=NE), basec[:], ALU.add, waits=[t, t_win])
                t = P.op("vector", "tensor_tensor", cnt[:], basec[:, NT - 1, :], tot[:, NT - 1, :], ALU.add, waits=[t])
                cA = cnt[:].unsqueeze(2).to_broadcast([128, NE, NE])
                cB = cnt[:].unsqueeze(1).to_broadcast([128, NE, NE])
                t = P.op("vector", "tensor_tensor", cm1, cB, cA, ALU.is_gt, waits=[t])
                t = P.op("vector", "tensor_tensor", cm2, cB, cA, ALU.is_equal, waits=[t])
                t = P.op("vector", "tensor_tensor", cm2, cm2, ltm[:], ALU.mult, waits=[t, t_c])
                t = P.op("vector", "tensor_tensor", cm1, cm1, cm2, ALU.add, waits=[t])
                t = P.op("vector", "tensor_reduce", rank[:], cm1, AX.X, ALU.add, waits=[t])
                iota_j = tab[:, 0:NE].unsqueeze(1).to_broadcast([128, NE, NE])
                t_ohr = P.op("vector", "tensor_tensor", oh_r[:], rank[:].unsqueeze(2).to_broadcast([128, NE, NE]), iota_j, ALU.is_equal, waits=[t])
                t = P.op("vector", "tensor_tensor", cm1, oh_r[:], tab[:, NE:2 * NE].unsqueeze(1).to_broadcast([128, NE, NE]), ALU.mult, waits=[t_ohr])
                t = P.op("vector", "tensor_reduce", base_e[:], cm1, AX.X, ALU.add, waits=[t])
                t = P.op("vector", "tensor_tensor", cm2, oh_r[:], tab[:, 2 * NE:3 * NE].unsqueeze(1).to_broadcast([128, NE, NE]), ALU.mult, waits=[t])
                t = P.op("vector", "tensor_reduce", cap_e[:], cm2, AX.X, ALU.add, waits=[t])
                t = P.op("vector", "tensor_tensor", cm1, oh_r[:].rearrange("p e j -> p j e"), iota_j, ALU.mult, waits=[t])
                t = P.op("vector", "tensor_reduce", perm[:], cm1, AX.X, ALU.add, waits=[t])
                t_ib = P.op("vector", "tensor_copy", idxb[:], perm[:], waits=[t])
                t = P.op("vector", "tensor_scalar_mul", perm[:], perm[:], 1024.0, waits=[t_ib])
                t = P.op("vector", "tensor_tensor", idxwf[:], perm[:].unsqueeze(2).to_broadcast([128, NE, 8]),
                         iotacp[:].unsqueeze(1).to_broadcast([128, NE, 8]), ALU.add, waits=[t])
                t_iw = P.op("vector", "tensor_copy", idxw[:], idxwf[:], waits=[t])
                t = P.op("vector", "tensor_tensor", dest[:], pos[:], base_e[:].unsqueeze(1).to_broadcast([128, NT, NE]), ALU.add, waits=[t_iw])
                t = P.op("vector", "tensor_tensor", ovff[:], pos[:], cap_e[:].unsqueeze(1).to_broadcast([128, NT, NE]), ALU.is_ge, waits=[t])
                for k in range(4):
                    t = P.op("vector", "tensor_tensor", tmp3[:], oh[k][:], dest[:], ALU.mult, waits=[t])
                    t = P.op("vector", "tensor_reduce", destk[:, k, :], tmp3[:], AX.X, ALU.add, waits=[t])
                    t = P.op("vector", "tensor_tensor", tmp3[:], oh[k][:], ovff[:], ALU.mult, waits=[t])
                    t = P.op("vector", "tensor_reduce", ovf[:, k, :], tmp3[:], AX.X, ALU.add, waits=[t])
                t = P.op("vector", "tensor_scalar", posk[:], ovf[:], trash[:, 0:1], None, ALU.mult, waits=[t, t_c])
                t = P.op("vector", "tensor_scalar", ovf[:], ovf[:], -1.0, 1.0, ALU.mult, ALU.add, waits=[t])
                t = P.op("vector", "tensor_tensor", destk[:], destk[:], ovf[:], ALU.mult, waits=[t])
                t = P.op("vector", "tensor_tensor", destk[:], destk[:], posk[:], ALU.add, waits=[t])
                t_idx = P.op("vector", "tensor_copy", idx[:], destk[:], waits=[t])
                t = P.op("vector", "tensor_tensor", gates[:], gates[:], ovf[:], ALU.mult, waits=[t_idx, t_g])
                n = 0
                t_sc = [None] * 4
                for i in range(NT):
                    for k in range(4):
                        t_sc[n % 4] = P.dma("gpsimd", d_sc[n % 4], xd_d[:, :], h3_all[:, i, :], waits=[t_idx, t_sc[n % 4]],
                                            meth="indirect_dma_start",
                                            out_offset=bass.IndirectOffsetOnAxis(ap=idx[:, k, i:i + 1], axis=0), in_offset=None)
                        n += 1
                P.wait("gpsimd", t_sc)
                P.wait("sync", [t_x2])
                P.wait("vector", [t])
                P.run()
            if stage == 4:
                pdg = ExitStack()
                with pdg:
                    P = Prog(nc, pdg)
                    d_o = P.dsem()
                    idxf = sb("idxf", [128, 4 * NT], F32, pdg)
                    t = P.op("vector", "tensor_copy", idxf[:], idx[:].rearrange("p a b -> p (a b)"))
                    P.dma("sync", d_o, dbg["x"][0:128, 0:64], idxf[:], waits=[t])
                    t2 = P.dma("sync", d_o, dbg["x"][128:256, 0:64], gates[:].rearrange("p a b -> p (a b)"))
                    P.wait("sync", [t2])
                    P.run()
                return nc

            pe = ExitStack()
            with pe:
                P = Prog(nc, pe)
                xe_tok = [sb(f"xe_tok{i}", [128, NST, D], BF16, pe) for i in range(2)]
                xeT = [sb(f"xeT{i}", [128, 8, CAP], BF16, pe) for i in range(2)]
                hbT = [sb(f"hbT{i}", [128, 8, CAP], BF16, pe) for i in range(2)]
                ye_ring = Ring([sb(f"ye{i}", [128, D], F32, pe) for i in range(3)])
                bd = [sb(f"bd{i}", [128, D], F32, pe) for i in range(2)]
                bsel = [sb(f"bsel{i}", [128, 16], F32, pe) for i in range(2)]
                tmpb = sb("tmpb", [128, NE, 16], F32, pe)
                g1 = [sb(f"g1_{i}", [128, CAP], F32, pe) for i in range(2)]
                sg = [sb(f"sg_{i}", [128, CAP], F32, pe) for i in range(2)]
                u1 = [sb(f"u1_{i}", [128, CAP], F32, pe) for i in range(2)]
                tpx = [ps(f"tpx{i}", [128, 8, 128], BF16, pe) for i in range(2)]
                gu_ring = Ring([ps(f"gu_ps{i}", [128, 512], F32, pe) for i in range(4)])
                dn_ring = Ring([ps(f"dn_ps{i}", [128, 512], F32, pe) for i in range(2)])
                d_xe = [P.dsem() for _ in range(2)]
                d_y = [P.dsem() for _ in range(3)]
                t_wgu_free = [None, None]
                t_wd_free = [None, None]
                t_xe_free = [None, None]
                t_bd_free = [None, None]
                t_xeT_free = [None, None]
                t_hbT_free = [None, None]
                t_tpx_free = [None, None]
                t_g1_free = [None, None]
                t_sg_free = [None, None]
                t_u1_free = [None, None]
                t_bsel_free = [None, None]
                ntp = 0
                nsw = 0
                loads = {}
                bgu3 = bgu_sb[:].rearrange("p (e c) -> p e c", c=16)
                t_tmpb_free = None

                def issue_loads(e):
                    sl = e % 2
                    nt_ = ITEM_TILES[e]
                    if e not in wtok:
                        issue_wloads(P, e, waits_g=[t_wgu_free[sl]], waits_d=[t_wd_free[sl]])
                    t_g, t_d = wtok[e]
                    t_b = P.dma("gpsimd", d_bdw[sl], bd[sl][:], b_down_d[:, :], waits=[t_bd_free[sl]],
                                meth="indirect_dma_start", out_offset=None,
                                in_offset=bass.IndirectOffsetOnAxis(ap=idxb[:, e:e + 1], axis=0))
                    t_x = P.dma("sync", d_xe[sl], xe_tok[sl][:, 0:nt_, :],
                                xd_d[ITEM_BASE[e]:ITEM_BASE[e] + nt_ * 128, :].rearrange("(s p) d -> p s d", p=128),
                                waits=[t_xe_free[sl]])
                    loads[e] = (t_g, t_d, t_x, t_b)

                issue_loads(0)
                issue_loads(1)
                t_yd = []
                for e in range(NE):
                    sl = e % 2
                    cap = PROFILE[e]
                    nt_ = ITEM_TILES[e]
                    t_g, t_d, t_x, t_b = loads[e]
                    t_tb = P.op("vector", "tensor_tensor", tmpb[:], bgu3, oh_r[:, :, e:e + 1].to_broadcast([128, NE, 16]), ALU.mult,
                                waits=[t_tmpb_free])
                    t_bs = P.op("vector", "tensor_reduce", bsel[sl][:], tmpb[:].rearrange("p e c -> p c e"), AX.X, ALU.add,
                                waits=[t_tb, t_bsel_free[sl]])
                    t_tmpb_free = t_bs
                    t_xT = []
                    t_tp = None
                    for st in range(nt_):
                        tb_ = ntp % 2
                        ntp += 1
                        for c in range(8):
                            t_tp = P.op("tensor", "transpose", out=tpx[tb_][:, c, :], in_=xe_tok[sl][:, st, c * 128:(c + 1) * 128],
                                        identity=ident_b[:], waits=[t_x, t_tpx_free[tb_]], sig=(c == 7))
                        t_cp = P.op("scalar", "activation", out=xeT[sl][:, :, st * 128:(st + 1) * 128], in_=tpx[tb_][:], func=AF.Copy,
                                    waits=[t_tp, t_xeT_free[sl]])
                        t_tpx_free[tb_] = t_cp
                        t_xT.append(t_cp)
                    t_xe_free[sl] = t_tp
                    t_hb = None
                    t_gu_last = None
                    for fc in range(8):
                        toks = []
                        bufs = []
                        for half in range(2):
                            col = half * D + fc * 128
                            bi, buf, fr = gu_ring.get()
                            t_m = None
                            for c in range(8):
                                t_m = P.op("tensor", "matmul", buf[:, 0:cap], lhsT=wgu[sl][:, c, col:col + 128], rhs=xeT[sl][:, c, 0:cap],
                                           start=(c == 0), stop=(c == 7), waits=t_xT + [t_g, fr], sig=(c == 7))
                            toks.append(t_m)
                            bufs.append((bi, buf))
                        t_gu_last = toks[1]
                        w = nsw % 2
                        nsw += 1
                        bgc = bsel[sl][:, fc:fc + 1]
                        buc = bsel[sl][:, 8 + fc:8 + fc + 1]
                        t1 = P.op("vector", "tensor_scalar", g1[w][:, 0:cap], bufs[0][1][:, 0:cap], bgc, SWIGLU_LIMIT, ALU.add, ALU.min,
                                  waits=[toks[0], t_g1_free[w], t_bs])
                        gu_ring.release(bufs[0][0], t1)
                        t2 = P.op("scalar", "activation", out=sg[w][:, 0:cap], in_=g1[w][:, 0:cap], func=AF.Sigmoid, scale=SWIGLU_ALPHA,
                                  waits=[t1, t_sg_free[w]])
                        t3 = P.op("vector", "tensor_scalar", u1[w][:, 0:cap], bufs[1][1][:, 0:cap], buc, SWIGLU_LIMIT, ALU.add, ALU.min,
                                  waits=[toks[1], t_u1_free[w]])
                        gu_ring.release(bufs[1][0], t3)
                        t4 = P.op("vector", "tensor_scalar", u1[w][:, 0:cap], u1[w][:, 0:cap], -SWIGLU_LIMIT, 1.0, ALU.max, ALU.add, waits=[t3])
                        t5 = P.op("vector", "tensor_tensor", g1[w][:, 0:cap], g1[w][:, 0:cap], sg[w][:, 0:cap], ALU.mult, waits=[t2])
                        t_sg_free[w] = t5
                        t_hb = P.op("vector", "tensor_tensor", hbT[sl][:, fc, 0:cap], g1[w][:, 0:cap], u1[w][:, 0:cap], ALU.mult,
                                    waits=[t5, t4, t_hbT_free[sl]])
                        t_g1_free[w] = t_hb
                        t_u1_free[w] = t_hb
                    t_wgu_free[sl] = t_gu_last
                    t_xeT_free[sl] = t_gu_last
                    t_bsel_free[sl] = t_hb
                    t_ev = None
                    t_dn = None
                    for st in range(nt_):
                        m_ = min(128, cap - st * 128)
                        yi, ybuf, yfr = ye_ring.get()
                        for hf in range(2):
                            bi, buf, fr = dn_ring.get()
                            for fc in range(8):
                                t_dn = P.op("tensor", "matmul", buf[0:m_, :], lhsT=hbT[sl][:, fc, st * 128:st * 128 + m_],
                                            rhs=wd[sl][:, fc, hf * 512:(hf + 1) * 512], start=(fc == 0), stop=(fc == 7),
                                            waits=[t_hb, t_d, fr], sig=(fc == 7))
                            t_ev = P.op("vector", "tensor_tensor", ybuf[0:m_, hf * 512:(hf + 1) * 512], buf[0:m_, :],
                                        bd[sl][0:m_, hf * 512:(hf + 1) * 512], ALU.add, waits=[t_dn, t_b, yfr])
                            dn_ring.release(bi, t_ev)
                        r0 = ITEM_BASE[e] + st * 128
                        t_st = P.dma("sync", d_y[yi], yd_d[r0:r0 + m_, :], ybuf[0:m_, :], waits=[t_ev])
                        ye_ring.release(yi, t_st)
                        t_yd.append(t_st)
                    t_wd_free[sl] = t_dn
                    t_hbT_free[sl] = t_dn
                    t_bd_free[sl] = t_ev
                    if e + 2 < NE:
                        issue_loads(e + 2)
                P.wait("sync", t_yd[-3:])
                P.run()

            pf = ExitStack()
            with pf:
                P = Prog(nc, pf)
                gf_bc = sb("gf_bc", [128, D], F32, pf)
                NB = 4
                yk = [[x_res[:, 4 * i + k, :] for k in range(4)] for i in range(NB)]
                xa = [sb(f"xa{i}", [128, D], F32, pf)[:] for i in range(NB)]
                ot = [sb(f"ot{i}", [128, D], F32, pf) for i in range(2)]
                junk = sb("junk5", [128, D], BF16, pf)
                fss = sb("fss", [128, NT], F32, pf)
                frstd = sb("frstd", [128, NT], F32, pf)
                d_c = P.dsem()
                d_g = [[P.dsem() for k in range(4)] for i in range(NB)]
                d_xa = [P.dsem() for _ in range(NB)]
                d_out = [P.dsem() for _ in range(2)]
                t_c = P.dma("sync", d_c, gf_bc[:], final_g_d.partition_broadcast(128))
                t_yk_free = [None] * NB
                t_xa_free = [None] * NB
                t_ot_free = [None, None]
                t_outs = [None, None]
                ld = {}

                def stLoad(i):
                    b = i % NB
                    t_x = P.dma("sync", d_xa[b], xa[b], x2_d[i * 128:(i + 1) * 128, :], waits=[t_xa_free[b]])
                    t_gk = []
                    for k in range(4):
                        t_gk.append(P.dma("gpsimd", d_g[b][k], yk[b][k], yd_d[:, :], waits=[t_yk_free[b]],
                                          meth="indirect_dma_start", out_offset=None,
                                          in_offset=bass.IndirectOffsetOnAxis(ap=idx[:, k, i:i + 1], axis=0)))
                    ld[i] = (t_x, t_gk)

                def stComb(i):
                    b = i % NB
                    ob = i % 2
                    t_x, t_gk = ld[i]
                    t = t_x
                    for k in range(4):
                        t = P.op("vector", "scalar_tensor_tensor", out=xa[b], in0=yk[b][k], scalar=gates[:, k, i:i + 1],
                                 in1=xa[b], op0=ALU.mult, op1=ALU.add, waits=[t, t_gk[k]])
                    t_yk_free[b] = t
                    t_ss = P.op("scalar", "activation", out=junk[:], in_=xa[b], func=AF.Square, accum_out=fss[:, i:i + 1], waits=[t])
                    t_r = _rstd(P, fss[:, i:i + 1], frstd[:, i:i + 1], D, [t_ss], eps_t[:])
                    t_o = P.op("vector", "scalar_tensor_tensor", out=ot[ob][:], in0=xa[b], scalar=frstd[:, i:i + 1], in1=gf_bc[:],
                               op0=ALU.mult, op1=ALU.mult, waits=[t_r, t_c, t_ot_free[ob]])
                    t_xa_free[b] = t_o
                    t_outs[ob] = P.dma("sync", d_out[ob], out_d[i * 128:(i + 1) * 128, :], ot[ob][:], waits=[t_o])
                    t_ot_free[ob] = t_outs[ob]

                for i in range(NB - 1):
                    stLoad(i)
                for i in range(NT):
                    if i + NB - 1 < NT:
                        stLoad(i + NB - 1)
                    stComb(i)
                P.wait("sync", t_outs)
                P.run()
    return nc


def host_consts():
    c = {}
    c["c_ident"] = np.eye(128, dtype=np.float32)
    c["c_invf"] = (500000.0 ** (-np.arange(0, 16, 2, dtype=np.float32) / 16)).astype(np.float32).reshape(1, 8)
    pm = np.zeros((3, 128, 4, 128), np.float32)
    wins = (2, 4, 8, 16)
    for g, w in enumerate(wins):
        for t in range(128):
            lo = max(t + 1 - w, 0)
            cnt = t + 1 - lo
            pm[0, lo:t + 1, g, t] = 1.0 / cnt
            pm[0, t, g, t] -= 1.0
            for tp in range(t + 1 - w, t + 1):
                if tp >= 0:
                    pm[1, tp, g, t] = 1.0 / w
                else:
                    pm[2, 128 + tp, g, t] = 1.0 / w
            pm[1, t, g, t] -= 1.0
    c["c_poolm"] = pm
    c["c_tri"] = np.triu(np.ones((128, 128), np.float32), 1)
    c["c_eoff"] = (np.arange(NE, dtype=np.float32) * CAP).reshape(1, NE)
    c["c_trash"] = (NROWS + np.arange(128, dtype=np.float32)).reshape(128, 1)
    c["c_lt"] = np.tril(np.ones((NE, NE), np.float32), -1).reshape(1, NE * NE)
    c["c_tab"] = np.concatenate([np.arange(NE, dtype=np.float32), np.asarray(ITEM_BASE, np.float32),
                                 np.asarray(PROFILE, np.float32)]).reshape(1, 3 * NE)
    c["c_iotacp"] = (np.arange(8, dtype=np.float32)[None, :] * 128 + np.arange(128, dtype=np.float32)[:, None])
    return c


def make_in_maps(inputs):
    f = lambda a: np.ascontiguousarray(np.asarray(a))
    shared = {
        "attn_norm_g": f(inputs["attn_norm_g"]).reshape(1, D),
        "w_in": f(inputs["w_in"][0]),
        "w_pool": f(inputs["w_pool"][0]),
        "pool_scale": f(np.asarray(inputs["pool_scale"][0]).reshape(4, 128).T),
        "lam4": f(np.stack([np.asarray(inputs[k][0]) for k in ("lambda_q1", "lambda_k1", "lambda_q2", "lambda_k2")])).reshape(1, 256),
        "subln_g": f(inputs["subln_g"]).reshape(1, 128),
        "w_out": f(inputs["w_out"][0]),
        "xattn_norm_g": f(inputs["xattn_norm_g"]).reshape(1, D),
        "mem_norm_g": f(inputs["mem_norm_g"]).reshape(1, D),
        "w_cq": f(inputs["w_cq"][0]),
        "w_ckv": f(inputs["w_ckv"][0]),
        "w_co": f(inputs["w_co"][0]),
        "ffn_norm_g": f(inputs["ffn_norm_g"]).reshape(1, D),
        "w_router": f(inputs["w_router"][0]),
        "b_router": f(inputs["b_router"]).reshape(1, NE),
        "w_gu": f(inputs["w_gu"][0]),
        "b_gu": f(inputs["b_gu"][0]).reshape(NE * 16, 128),
        "w_down": f(inputs["w_down"][0]),
        "b_down": f(inputs["b_down"][0]),
        "final_norm_g": f(inputs["final_norm_g"]).reshape(1, D),
    }
    shared.update(host_consts())
    maps = []
    for b in range(NCORES):
        m = dict(shared)
        m["x"] = f(inputs["x"][b])
        m["pos"] = f(np.asarray(inputs["positions"][b]).astype(np.int32).reshape(NT, 128).T)
        m["mem"] = f(inputs["mem"][b])
        maps.append(m)
    return maps


def kernel(**inputs):
    nc = build()
    maps = make_in_maps(inputs)
    res = run_bass_kernel_spmd(nc, maps, core_ids=list(range(NCORES)))
    return np.stack([r["out"] for r in res.results], axis=0).astype(np.float32)
```
